# Optimizing a Trainium2 kernel written in Bass

```python
import jax, jax.numpy as jnp
from jax import lax
import numpy as np

D_MODEL = 1024
BATCH = 16
SEQ = 2048
DEPTH = 4

GRID_W = 64
CTX_LEN = 256
HEAD_DIM = 64
ROPE_BASE = 10000.0
Q_BLOCK = 128
NORM_EPS = 1e-6
LN_EPS = 1e-5
CONV_CH = D_MODEL // 2
CONV_WIDTH = 31
GQA_HEADS = (D_MODEL // 2) // HEAD_DIM
GQA_KV_HEADS = GQA_HEADS // 4
GQA_GROUP = GQA_HEADS // GQA_KV_HEADS
GQA_Q_W = GQA_HEADS * HEAD_DIM
GQA_KV_W = GQA_KV_HEADS * HEAD_DIM
EVEN_IN = 2 * CONV_CH + GQA_Q_W + 2 * GQA_KV_W
EVEN_MIX = CONV_CH + GQA_Q_W
DIFF_HEADS = D_MODEL // (2 * HEAD_DIM)
DIFF_V_DIM = 2 * HEAD_DIM
DIFF_QK_W = DIFF_HEADS * 2 * HEAD_DIM
ODD_IN = 3 * DIFF_QK_W
FFN_DIM = (7 * D_MODEL) // 2
N_EXPERTS = 8
TOP_K = 2
N_EVEN = (DEPTH + 1) // 2
N_ODD = DEPTH // 2

kernel_name = 'hybrid_conv_gqa_diffattn_moe_dit'


def _rms_norm(x, g):
    xf = x.astype(jnp.float32)
    y = xf * lax.rsqrt(jnp.mean(xf * xf, axis=-1, keepdims=True) + NORM_EPS)
    return (y * g.astype(jnp.float32)).astype(x.dtype)


def _layer_norm(x, g, b):
    xf = x.astype(jnp.float32)
    mu = jnp.mean(xf, axis=-1, keepdims=True)
    var = jnp.mean(jnp.square(xf - mu), axis=-1, keepdims=True)
    y = (xf - mu) * lax.rsqrt(var + LN_EPS) * g.astype(jnp.float32) + b.astype(jnp.float32)
    return y.astype(x.dtype)


def _axial_rope_tables(n_lat):
    rows = n_lat // GRID_W
    r = jnp.broadcast_to(jnp.arange(rows, dtype=jnp.float32)[:, None], (rows, GRID_W)).reshape(-1)
    col = jnp.broadcast_to(jnp.arange(GRID_W, dtype=jnp.float32)[None, :], (rows, GRID_W)).reshape(-1)
    quarter = HEAD_DIM // 4
    inv_freq = ROPE_BASE ** (-jnp.arange(quarter, dtype=jnp.float32) / quarter)
    ang = jnp.stack([r[:, None] * inv_freq, col[:, None] * inv_freq], axis=1)
    return jnp.cos(ang), jnp.sin(ang)


def _apply_rope(x, cos, sin):
    shp = x.shape
    quarter = HEAD_DIM // 4
    xf = x.astype(jnp.float32).reshape(shp[:-1] + (2, 2, quarter))
    bshape = (1, shp[1]) + (1,) * (len(shp) - 3) + (2, quarter)
    cb = cos.reshape(bshape)
    sb = sin.reshape(bshape)
    x1 = xf[..., 0, :]
    x2 = xf[..., 1, :]
    out = jnp.stack([x1 * cb - x2 * sb, x2 * cb + x1 * sb], axis=-2)
    return out.reshape(shp).astype(x.dtype)


def _sweep_query_blocks(fn, q):
    bsz, n = q.shape[0], q.shape[1]
    nb = n // Q_BLOCK
    qb = jnp.moveaxis(q.reshape((bsz, nb, Q_BLOCK) + q.shape[2:]), 1, 0)
    out = jnp.moveaxis(lax.map(fn, qb), 0, 1)
    return out.reshape((bsz, n) + out.shape[3:])


def _gqa_block(qb, k, v):
    s = jnp.einsum('bqkgd,bskd->bkgqs', qb, k).astype(jnp.float32) * (HEAD_DIM ** -0.5)
    p = jax.nn.softmax(s, axis=-1).astype(v.dtype)
    return jnp.einsum('bkgqs,bskd->bqkgd', p, v)


def _diff_block(qb, k, v, lam):
    s = jnp.einsum('bqhcd,bshcd->bhcqs', qb, k).astype(jnp.float32) * (HEAD_DIM ** -0.5)
    p = jax.nn.softmax(s, axis=-1)
    a = p[:, :, 0] - lam * p[:, :, 1]
    return jnp.einsum('bhqs,bshe->bqhe', a.astype(v.dtype), v)


def _conv_branch(a, g, conv_w, ln_g, ln_b):
    y = a * jax.nn.sigmoid(g)
    pad = CONV_WIDTH // 2
    y = lax.conv_general_dilated(y, conv_w[:, None, :].astype(y.dtype), (1,), [(pad, pad)],
                                 dimension_numbers=('NWC', 'WIO', 'NWC'), feature_group_count=CONV_CH)
    return jax.nn.silu(_layer_norm(y, ln_g, ln_b))


def _even_mixer(h_lat, h_ctx, w_in, conv_w, ln_g, ln_b, qn_g, kn_g, w_out, cos, sin, need_ctx):
    bsz, n_lat, _ = h_lat.shape
    n_ctx = h_ctx.shape[1]
    cuts = [CONV_CH, 2 * CONV_CH, 2 * CONV_CH + GQA_Q_W, 2 * CONV_CH + GQA_Q_W + GQA_KV_W]
    a_l, g_l, q_l, k_l, v_l = jnp.split(h_lat @ w_in, cuts, axis=-1)
    a_c, g_c, q_c, k_c, v_c = jnp.split(h_ctx @ w_in, cuts, axis=-1)
    q_l = _apply_rope(_rms_norm(q_l.reshape(bsz, n_lat, GQA_HEADS, HEAD_DIM), qn_g), cos, sin)
    k_l = _apply_rope(_rms_norm(k_l.reshape(bsz, n_lat, GQA_KV_HEADS, HEAD_DIM), kn_g), cos, sin)
    v_l = v_l.reshape(bsz, n_lat, GQA_KV_HEADS, HEAD_DIM)
    k_c = _rms_norm(k_c.reshape(bsz, n_ctx, GQA_KV_HEADS, HEAD_DIM), kn_g)
    v_c = v_c.reshape(bsz, n_ctx, GQA_KV_HEADS, HEAD_DIM)
    keys = jnp.concatenate([k_l, k_c], axis=1)
    vals = jnp.concatenate([v_l, v_c], axis=1)
    q_l = q_l.reshape(bsz, n_lat, GQA_KV_HEADS, GQA_GROUP, HEAD_DIM)
    att_l = _sweep_query_blocks(lambda qb: _gqa_block(qb, keys, vals), q_l).reshape(bsz, n_lat, GQA_Q_W)
    conv_l = _conv_branch(a_l, g_l, conv_w, ln_g, ln_b)
    y_l = jnp.concatenate([conv_l, att_l], axis=-1) @ w_out
    y_c = None
    if need_ctx:
        q_c = _rms_norm(q_c.reshape(bsz, n_ctx, GQA_HEADS, HEAD_DIM), qn_g)
        q_c = q_c.reshape(bsz, n_ctx, GQA_KV_HEADS, GQA_GROUP, HEAD_DIM)
        att_c = _gqa_block(q_c, k_c, v_c).reshape(bsz, n_ctx, GQA_Q_W)
        conv_c = _conv_branch(a_c, g_c, conv_w, ln_g, ln_b)
        y_c = jnp.concatenate([conv_c, att_c], axis=-1) @ w_out
    return y_l, y_c


def _odd_mixer(h_lat, h_ctx, w_in, lam_p, subln_g, w_out, cos, sin, lam_init, need_ctx):
    def proj(h):
        b, n, _ = h.shape
        q, k, v = jnp.split(h @ w_in, [DIFF_QK_W, 2 * DIFF_QK_W], axis=-1)
        return (q.reshape(b, n, DIFF_HEADS, 2, HEAD_DIM), k.reshape(b, n, DIFF_HEADS, 2, HEAD_DIM),
                v.reshape(b, n, DIFF_HEADS, DIFF_V_DIM))

    q_l, k_l, v_l = proj(h_lat)
    q_c, k_c, v_c = proj(h_ctx)
    q_l = _apply_rope(q_l, cos, sin)
    k_l = _apply_rope(k_l, cos, sin)
    lp = lam_p.astype(jnp.float32)
    lam = jnp.exp(jnp.sum(lp[0] * lp[1])) - jnp.exp(jnp.sum(lp[2] * lp[3])) + lam_init
    keys = jnp.concatenate([k_l, k_c], axis=1)
    vals = jnp.concatenate([v_l, v_c], axis=1)

    def finish(o):
        o = _rms_norm(o, subln_g) * (1.0 - lam_init)
        return o.reshape(o.shape[0], o.shape[1], DIFF_HEADS * DIFF_V_DIM) @ w_out

    y_l = finish(_sweep_query_blocks(lambda qb: _diff_block(qb, keys, vals, lam), q_l))
    y_c = finish(_diff_block(q_c, k_c, v_c, lam)) if need_ctx else None
    return y_l, y_c


def _swiglu(h, wg, wu, wd):
    return (jax.nn.silu(h @ wg) * (h @ wu)) @ wd


def _moe(h, router_w, wg, wu, wd):
    logits = (h @ router_w).astype(jnp.float32)
    top_v, top_i = lax.top_k(logits, TOP_K)
    top_w = jax.nn.softmax(top_v, axis=-1)
    combine = jnp.sum(jax.nn.one_hot(top_i, N_EXPERTS, dtype=jnp.float32) * top_w[..., None], axis=-2)
    y = jnp.zeros_like(h)
    for e in range(N_EXPERTS):
        y = y + combine[..., e:e + 1].astype(h.dtype) * _swiglu(h, wg[e], wu[e], wd[e])
    return y


def setup_inputs(seed: int = 0) -> dict:
    key = jax.random.key(seed)
    ks = iter(jax.random.split(key, 32))
    f32 = jnp.float32
    D, F = D_MODEL, FFN_DIM

    def nrm(shape, scale):
        return jax.random.normal(next(ks), shape, f32) * scale

    def gain(shape):
        return 1.0 + nrm(shape, 0.02)

    return {
        'x': nrm((BATCH, SEQ, D), 1.0),
        'c': nrm((BATCH, D), 1.0),
        'ctx': nrm((BATCH, CTX_LEN, D), 1.0),
        'c_ctx': nrm((D,), 1.0),
        'ada_w': nrm((DEPTH, D, 6 * D), 0.5 * D ** -0.5),
        'ada_b': nrm((DEPTH, 6 * D), 0.02),
        'norm1_g': gain((DEPTH, D)),
        'norm2_g': gain((DEPTH, D)),
        'ev_w_in': nrm((N_EVEN, D, EVEN_IN), D ** -0.5),
        'ev_conv_w': nrm((N_EVEN, CONV_WIDTH, CONV_CH), CONV_WIDTH ** -0.5),
        'ev_ln_g': gain((N_EVEN, CONV_CH)),
        'ev_ln_b': nrm((N_EVEN, CONV_CH), 0.02),
        'ev_q_norm_g': gain((N_EVEN, HEAD_DIM)),
        'ev_k_norm_g': gain((N_EVEN, HEAD_DIM)),
        'ev_w_out': nrm((N_EVEN, EVEN_MIX, D), EVEN_MIX ** -0.5),
        'ev_ffn_wg': nrm((N_EVEN, D, F), D ** -0.5),
        'ev_ffn_wu': nrm((N_EVEN, D, F), D ** -0.5),
        'ev_ffn_wd': nrm((N_EVEN, F, D), F ** -0.5),
        'od_w_in': nrm((N_ODD, D, ODD_IN), D ** -0.5),
        'od_lam': nrm((N_ODD, 4, HEAD_DIM), 0.1),
        'od_subln_g': gain((N_ODD, DIFF_V_DIM)),
        'od_w_out': nrm((N_ODD, DIFF_HEADS * DIFF_V_DIM, D), (DIFF_HEADS * DIFF_V_DIM) ** -0.5),
        'od_router_w': nrm((N_ODD, D, N_EXPERTS), D ** -0.5),
        'od_moe_wg': nrm((N_ODD, N_EXPERTS, D, F), D ** -0.5),
        'od_moe_wu': nrm((N_ODD, N_EXPERTS, D, F), D ** -0.5),
        'od_moe_wd': nrm((N_ODD, N_EXPERTS, F, D), F ** -0.5),
        'final_norm_g': gain((D,)),
    }


def reference(x, c, ctx, c_ctx, ada_w, ada_b, norm1_g, norm2_g, ev_w_in, ev_conv_w, ev_ln_g, ev_ln_b,
              ev_q_norm_g, ev_k_norm_g, ev_w_out, ev_ffn_wg, ev_ffn_wu, ev_ffn_wd, od_w_in, od_lam,
              od_subln_g, od_w_out, od_router_w, od_moe_wg, od_moe_wu, od_moe_wd, final_norm_g):
    n_lat = x.shape[1]
    cos, sin = _axial_rope_tables(n_lat)
    s_lat = jax.nn.silu(c)
    s_ctx = jax.nn.silu(c_ctx)
    xl, xc = x, ctx
    for l in range(DEPTH):
        need_ctx = l < DEPTH - 1
        i = l // 2
        m_l = jnp.split((s_lat @ ada_w[l] + ada_b[l])[:, None, :], 6, axis=-1)
        m_c = jnp.split((s_ctx @ ada_w[l] + ada_b[l])[None, None, :], 6, axis=-1)
        h_l = _rms_norm(xl, norm1_g[l]) * (1.0 + m_l[1]) + m_l[0]
        h_c = _rms_norm(xc, norm1_g[l]) * (1.0 + m_c[1]) + m_c[0]
        if l % 2 == 0:
            y_l, y_c = _even_mixer(h_l, h_c, ev_w_in[i], ev_conv_w[i], ev_ln_g[i], ev_ln_b[i],
                                   ev_q_norm_g[i], ev_k_norm_g[i], ev_w_out[i], cos, sin, need_ctx)
        else:
            lam_init = 0.8 - 0.6 * float(np.exp(-0.3 * l))
            y_l, y_c = _odd_mixer(h_l, h_c, od_w_in[i], od_lam[i], od_subln_g[i], od_w_out[i],
                                  cos, sin, lam_init, need_ctx)
        xl = xl + m_l[2] * y_l
        if need_ctx:
            xc = xc + m_c[2] * y_c
        h_l = _rms_norm(xl, norm2_g[l]) * (1.0 + m_l[4]) + m_l[3]
        if l % 2 == 0:
            xl = xl + m_l[5] * _swiglu(h_l, ev_ffn_wg[i], ev_ffn_wu[i], ev_ffn_wd[i])
        else:
            xl = xl + m_l[5] * _moe(h_l, od_router_w[i], od_moe_wg[i], od_moe_wu[i], od_moe_wd[i])
        if need_ctx:
            h_c = _rms_norm(xc, norm2_g[l]) * (1.0 + m_c[4]) + m_c[3]
            if l % 2 == 0:
                xc = xc + m_c[5] * _swiglu(h_c, ev_ffn_wg[i], ev_ffn_wu[i], ev_ffn_wd[i])
            else:
                xc = xc + m_c[5] * _moe(h_c, od_router_w[i], od_moe_wg[i], od_moe_wu[i], od_moe_wd[i])
    return _rms_norm(xl, final_norm_g)
```

```python
import numpy as np
import concourse.bass as bass
import concourse.mybir as mybir
from concourse.bass_utils import run_bass_kernel_spmd

F32 = mybir.dt.float32
BF16 = mybir.dt.bfloat16
ALU = mybir.AluOpType
AF = mybir.ActivationFunctionType
AX = mybir.AxisListType
DT_SIZE = {F32: 4, BF16: 2}

D = 1024
KC = 8
TL = 2048
TC = 256
T = TL + TC
NT = T // 128
NTL = TL // 128
FF = 3584
NFC = FF // 128
NE = 8
HD = 64
DEPTH = 4
NB = 2
EVEN_IN = 1792
ODD_IN = 3072
CONVW = 31
PAD = 15
YT_W = PAD + TL + PAD + TC + PAD + 3
CTX_OFF = PAD + TL + PAD


class _Eng:
    def __init__(self, name, sem, unit):
        self.name = name
        self.sem = sem
        self.unit = unit
        self.count = 0
        self.thunks = []
        self.waited = {}


class Sched:
    REAL = ("pe", "act", "dve", "pool", "sp")

    def __init__(self, nc, n_dma_chan=28):
        self.nc = nc
        self.eng = {}
        self._sems = []
        for n in self.REAL:
            self.eng[n] = _Eng(n, self._new_sem("s_" + n), 1)
        self.chans = []
        for i in range(n_dma_chan):
            e = _Eng("dma%d" % i, self._new_sem("s_dma%d" % i), 16)
            self.eng[e.name] = e
            self.chans.append(e)
        self.next_chan = 0
        self.last_write = {}
        self.readers = {}

    def _new_sem(self, name):
        cm = self.nc.semaphore(name)
        s = cm.__enter__()
        self._sems.append(cm)
        return s

    def _deps(self, reads, writes):
        deps = {}

        def add(d):
            if d is None:
                return
            n, i = d
            if deps.get(n, 0) < i:
                deps[n] = i
        for r in reads:
            add(self.last_write.get(r))
        for w in writes:
            add(self.last_write.get(w))
            for n, i in self.readers.get(w, {}).items():
                add((n, i))
        return deps

    def _emit_waits(self, e, deps):
        for n, i in deps.items():
            if n == e.name and n == "pe":
                continue
            if e.waited.get(n, 0) >= i:
                continue
            e.waited[n] = i
            d = self.eng[n]
            val = i * d.unit
            sem = d.sem
            e.thunks.append(lambda o, sem=sem, val=val: o.wait_ge(sem, val))

    def _commit(self, name, idx, reads, writes):
        for r in reads:
            self.readers.setdefault(r, {})[name] = idx
        for w in writes:
            self.last_write[w] = (name, idx)
            self.readers[w] = {}

    def op(self, eng, fn, reads=(), writes=()):
        ps_r = [r for r in reads if isinstance(r, tuple) and r[0] == "ps"]
        if ps_r:
            writes = list(writes) + [r for r in ps_r if r not in writes]
        e = self.eng[eng]
        self._emit_waits(e, self._deps(reads, writes))
        e.count += 1
        sem = e.sem

        def thunk(o, fn=fn, sem=sem):
            ins = fn(o)
            ins.then_inc(sem, 1)
        e.thunks.append(thunk)
        self._commit(eng, e.count, reads, writes)

    def dma(self, queue, out, in_, reads=(), writes=(), **kw):
        q = self.eng[queue]
        ch = self.chans[self.next_chan]
        self.next_chan = (self.next_chan + 1) % len(self.chans)
        deps = self._deps(reads, writes)
        if ch.count > 0:
            deps[ch.name] = max(deps.get(ch.name, 0), ch.count)
        self._emit_waits(q, deps)
        ch.count += 1
        sem = ch.sem
        q.thunks.append(lambda o, out=out, in_=in_, sem=sem, kw=kw:
                        o.dma_start(out=out, in_=in_, **kw).then_inc(sem, 16))
        self._commit(ch.name, ch.count, reads, writes)

    def barrier(self):
        for n in self.REAL:
            e = self.eng[n]
            deps = {m: d.count for m, d in self.eng.items() if d.count > 0 and m != n}
            self._emit_waits(e, deps)

    def finish(self, final_eng="sp"):
        e = self.eng[final_eng]
        deps = {m: d.count for m, d in self.eng.items() if d.count > 0 and m != final_eng}
        self._emit_waits(e, deps)

    def emit(self):
        nc = self.nc
        with nc.Block() as block:
            @block.tensor
            def _(o):
                for t in self.eng["pe"].thunks:
                    t(o)

            @block.scalar
            def _(o):
                for t in self.eng["act"].thunks:
                    t(o)

            @block.vector
            def _(o):
                for t in self.eng["dve"].thunks:
                    t(o)

            @block.gpsimd
            def _(o):
                for t in self.eng["pool"].thunks:
                    t(o)

            @block.sync
            def _(o):
                for t in self.eng["sp"].thunks:
                    t(o)
        for cm in reversed(self._sems):
            cm.__exit__(None, None, None)


class Arena:
    def __init__(self, nc, nbytes):
        self.words = nbytes // 4
        self.cm = nc.sbuf_tensor("arena", [128, self.words], F32)
        self.t = self.cm.__enter__()
        self.top = 0
        self.peak = 0

    def mark(self):
        return self.top

    def release(self, m):
        self.top = m

    def alloc(self, shape, dtype):
        assert shape[0] == 128
        n = int(np.prod(shape[1:]))
        nw = (n * DT_SIZE[dtype] + 3) // 4
        nw = (nw + 7) // 8 * 8
        off = self.top
        self.top += nw
        assert self.top <= self.words, "arena overflow %d > %d" % (self.top * 4, self.words * 4)
        self.peak = max(self.peak, self.top)
        v = self.t[:, off:off + nw]
        if dtype != F32:
            v = v.bitcast(dtype)
        v = v[:, 0:n]
        if len(shape) > 2:
            names = " ".join("d%d" % i for i in range(len(shape) - 1))
            kw = {"d%d" % i: shape[i + 1] for i in range(len(shape) - 1)}
            v = v.rearrange("p (%s) -> p %s" % (names, names), **kw)
        return v

    def close(self):
        self.cm.__exit__(None, None, None)


def bc(ap, shape):
    return ap.broadcast_to(shape)


def build_program(n_layers=DEPTH, nb=NB, stop=None):
    nc = bass.Bass("TRN2", target_bir_lowering=False)

    def din(name, shape):
        return nc.dram_tensor(name, list(shape), F32, kind="ExternalInput").ap()

    x_d = din("x", [NB, TL, D])
    c_d = din("c", [NB, D])
    ctx_d = din("ctx", [NB, TC, D])
    cctx_d = din("c_ctx", [D])
    adaw_d = din("ada_w", [DEPTH, D, 6 * D])
    adab_d = din("ada_b", [DEPTH, 6 * D])
    n1g_d = din("norm1_g", [DEPTH, D])
    n2g_d = din("norm2_g", [DEPTH, D])
    evwin_d = din("ev_w_in", [2, D, EVEN_IN])
    evcw_d = din("ev_conv_w", [2, CONVW, 512])
    evlng_d = din("ev_ln_g", [2, 512])
    evlnb_d = din("ev_ln_b", [2, 512])
    evqg_d = din("ev_q_norm_g", [2, HD])
    evkg_d = din("ev_k_norm_g", [2, HD])
    evwout_d = din("ev_w_out", [2, D, D])
    evwg_d = din("ev_ffn_wg", [2, D, FF])
    evwu_d = din("ev_ffn_wu", [2, D, FF])
    evwd_d = din("ev_ffn_wd", [2, FF, D])
    odwin_d = din("od_w_in", [2, D, ODD_IN])
    odlam_d = din("od_lam", [2, 4, HD])
    odsg_d = din("od_subln_g", [2, 128])
    odwout_d = din("od_w_out", [2, D, D])
    odrw_d = din("od_router_w", [2, D, NE])
    odwg_d = din("od_moe_wg", [2, NE, D, FF])
    odwu_d = din("od_moe_wu", [2, NE, D, FF])
    odwd_d = din("od_moe_wd", [2, NE, FF, D])
    fng_d = din("final_norm_g", [D])
    cos_d = din("rope_cos", [TL, 32])
    sin_d = din("rope_sin", [TL, 32])
    out_d = nc.dram_tensor("out", [NB, TL, D], F32, kind="ExternalOutput").ap()
    mods_d = nc.dram_tensor("mods", [DEPTH, 3, 6 * D], F32, kind="Internal").ap()

    S = Sched(nc)
    ar = Arena(nc, 206 * 1024)
    pcm = nc.psum_tensor("ps", [128, 8, 512], F32)
    ps = pcm.__enter__()

    def psb(b):
        return ps[:, b, :]

    def psb16(b):
        return ps[:, b, :].bitcast(BF16)

    xl = ar.alloc([128, NT, D], F32)
    hT = ar.alloc([128, KC, T], BF16)
    idb = ar.alloc([128, 128], BF16)
    idf = ar.alloc([128, 128], F32)
    onesN = ar.alloc([128, 128], F32)
    cos_t = ar.alloc([128, NTL, 32], F32)
    sin_t = ar.alloc([128, NTL, 32], F32)
    stat = ar.alloc([128, 64], F32)
    comb = ar.alloc([128, NT, NE], F32)
    base_mark = ar.mark()

    S.op("dve", lambda o: o.memset(idf, 0.0), writes=["idf"])
    S.op("pool", lambda o: o.affine_select(out=idf, in_=idf, pattern=[[-1, 128]], compare_op=ALU.not_equal,
                                           fill=1.0, base=0, channel_multiplier=1), reads=["idf"], writes=["idf"])
    S.op("dve", lambda o: o.tensor_copy(out=idb, in_=idf), reads=["idf"], writes=["idb"])
    S.op("dve", lambda o: o.memset(onesN, 1.0 / 512.0), writes=["onesN"])
    S.dma("sp", cos_t, cos_d.rearrange("(j p) f -> p j f", p=128), writes=["cos"])
    S.dma("sp", sin_t, sin_d.rearrange("(j p) f -> p j f", p=128), writes=["sin"])
    S.barrier()

    def prologue():
        m = ar.mark()
        c8 = ar.alloc([128, 3 * 128], F32)
        sT = ar.alloc([128, KC, 4], F32)
        wst = [ar.alloc([128, KC, 512], F32) for _ in range(2)]
        modrow = ar.alloc([128, 6 * D], F32)
        biasr = ar.alloc([128, 6 * D], F32)
        g1r = ar.alloc([128, D], F32)
        g2r = ar.alloc([128, D], F32)
        c8v = c8.rearrange("p (a b) -> p a b", a=3)
        for j in range(3):
            src = cctx_d if j == 2 else c_d[j]
            S.dma("sp", c8v[0:8, j, :], src.rearrange("(k p) -> k p", p=128), writes=["c8"])
        for j in range(3):
            S.op("pe", lambda o, j=j: o.transpose(out=ps[:, 0, j * 8:(j + 1) * 8], in_=c8v[0:8, j, :],
                                                  identity=idf[0:8, 0:8]), reads=["c8"], writes=[("ps", 0)])
        for j in range(3):
            S.op("act", lambda o, j=j: o.activation(out=sT[:, :, j], in_=ps[:, 0, j * 8:(j + 1) * 8], func=AF.Silu),
                 reads=[("ps", 0)], writes=["sT"])
        for l in range(DEPTH):
            S.dma("sp", biasr[0:3, :], adab_d[l].partition_broadcast(3), writes=["biasr"])
            S.dma("sp", g1r[0:3, :], n1g_d[l].partition_broadcast(3), writes=["g1r"])
            S.dma("sp", g2r[0:3, :], n2g_d[l].partition_broadcast(3), writes=["g2r"])
            wv = adaw_d[l].rearrange("(k p) n -> p k n", p=128)
            for nb_ in range(12):
                slot = nb_ % 2
                S.dma("sp", wst[slot], wv[:, :, nb_ * 512:(nb_ + 1) * 512], writes=[("wst", slot)])
                bank = 1 + slot

                def mm(o, slot=slot, bank=bank):
                    r = None
                    for k in range(KC):
                        r = o.matmul(ps[0:3, bank, :], lhsT=sT[:, k, 0:3], rhs=wst[slot][:, k, :],
                                     start=(k == 0), stop=(k == KC - 1))
                    return r
                S.op("pe", mm, reads=["sT", ("wst", slot)], writes=[("ps", bank)])
                S.op("dve", lambda o, bank=bank, nb_=nb_: o.tensor_tensor(
                    out=modrow[0:3, nb_ * 512:(nb_ + 1) * 512], in0=ps[0:3, bank, :],
                    in1=biasr[0:3, nb_ * 512:(nb_ + 1) * 512], op=ALU.add),
                    reads=[("ps", bank), "biasr"], writes=["modrow"])
            for ch, gr in ((1, g1r), (4, g2r)):
                S.op("dve", lambda o, ch=ch, gr=gr: o.scalar_tensor_tensor(
                    out=modrow[0:3, ch * D:(ch + 1) * D], in0=modrow[0:3, ch * D:(ch + 1) * D], scalar=1.0,
                    in1=gr[0:3, :], op0=ALU.add, op1=ALU.mult),
                    reads=["modrow", "g1r", "g2r"], writes=["modrow"])
            S.dma("sp", mods_d[l], modrow[0:3, :], reads=["modrow"], writes=["mods"])
        S.barrier()
        ar.release(m)

    prologue()

    def load_mod(dst, l, cond, chunk):
        S.dma("sp", dst, mods_d[l, cond, chunk * D:(chunk + 1) * D].partition_broadcast(128),
              reads=["mods"], writes=[("t", id(dst))])

    def tiles_for(need_ctx):
        return list(range(NT if need_ctx else NTL))

    def blocks_for(need_ctx):
        bl = [(i * 512, 512) for i in range(4)]
        if need_ctx:
            bl.append((TL, TC))
        return bl

    def norm_phase(l, b, which, need_ctx, router=None):
        m = ar.mark()
        ch_shift, ch_scale = (0, 1) if which == 1 else (3, 4)
        A = [ar.alloc([128, D], F32) for _ in range(2)]
        Sh = [ar.alloc([128, D], F32) for _ in range(2)]
        junk = ar.alloc([128, D], F32)
        t1 = [ar.alloc([128, D], F32) for _ in range(2)]
        nconds = 2 if need_ctx else 1
        for ci in range(nconds):
            cond = b if ci == 0 else 2
            load_mod(A[ci], l, cond, ch_scale)
            load_mod(Sh[ci], l, cond, ch_shift)
        if router is None:
            hb = [ar.alloc([128, D], BF16) for _ in range(2)]
        else:
            hb = [ar.alloc([128, D], F32) for _ in range(2)]
            hTf = [ar.alloc([128, KC, 128], F32) for _ in range(2)]
            rw = ar.alloc([128, KC, NE], F32)
            lg = ar.alloc([128, NT, NE], F32)
            m8 = ar.alloc([128, NT, 8], F32)
            wgt = ar.alloc([128, NT, 4], F32)
            e1 = ar.alloc([128, NT, NE], F32)
            S.dma("sp", rw, odrw_d[router].rearrange("(k p) e -> p k e", p=128), writes=["rw"])
        S.op("dve", lambda o: o.memset(stat, 0.0), writes=["stat"])
        def stA(j):
            xj = xl[:, j, :]
            S.op("act", lambda o, xj=xj, j=j: o.activation(out=junk, in_=xj, func=AF.Square, accum_out=stat[:, j:j + 1]),
                 reads=[("xl", j), "stat"], writes=["junk", ("stat", j)])
            S.op("act", lambda o, j=j: o.activation(out=stat[:, 32 + j:33 + j], in_=stat[:, j:j + 1], func=AF.Sqrt,
                                                    bias=1e-6, scale=1.0 / D), reads=[("stat", j)], writes=[("stat2", j)])
            S.op("dve", lambda o, j=j: o.reciprocal(out=stat[:, 32 + j:33 + j], in_=stat[:, 32 + j:33 + j]),
                 reads=[("stat2", j)], writes=[("stat2", j)])

        def stB(j):
            ci = 0 if j < NTL else 1
            s2 = j % 2
            xj = xl[:, j, :]
            S.op("dve", lambda o, xj=xj, j=j, ci=ci, s2=s2: o.scalar_tensor_tensor(
                out=t1[s2], in0=xj, scalar=stat[:, 32 + j:33 + j], in1=A[ci], op0=ALU.mult, op1=ALU.mult),
                reads=[("xl", j), ("stat2", j), ("t", id(A[ci]))], writes=[("t1", s2)])
            S.op("dve", lambda o, ci=ci, s2=s2: o.tensor_tensor(out=hb[s2], in0=t1[s2], in1=Sh[ci], op=ALU.add),
                 reads=[("t1", s2), ("t", id(Sh[ci]))], writes=[("hb", s2)])
            if router is None:
                bank = s2
                pv = psb16(bank)[:, 0:D].rearrange("p (c t) -> p c t", c=KC)

                def tr(o, s2=s2, pv=pv):
                    r = None
                    for c in range(KC):
                        r = o.transpose(out=pv[:, c, :], in_=hb[s2][:, c * 128:(c + 1) * 128], identity=idb)
                    return r
                S.op("pe", tr, reads=[("hb", s2)], writes=[("ps", bank)])
            else:
                b0 = 2 * s2
                pv = ps[:, b0:b0 + 2, :].rearrange("p a (c t) -> p (a c) t", c=4)

                def tr(o, s2=s2, pv=pv):
                    r = None
                    for c in range(KC):
                        r = o.transpose(out=pv[:, c, :], in_=hb[s2][:, c * 128:(c + 1) * 128], identity=idf)
                    return r
                S.op("pe", tr, reads=[("hb", s2)], writes=[("ps", b0), ("ps", b0 + 1)])

        def stC(j):
            s2 = j % 2
            if router is None:
                bank = s2
                pv = psb16(bank)[:, 0:D].rearrange("p (c t) -> p c t", c=KC)
                S.op("act", lambda o, pv=pv, j=j: o.copy(out=hT[:, :, j * 128:(j + 1) * 128], in_=pv),
                     reads=[("ps", bank)], writes=[("hT", j)])
            else:
                b0 = 2 * s2
                pv = ps[:, b0:b0 + 2, :].rearrange("p a (c t) -> p (a c) t", c=4)
                S.op("act", lambda o, pv=pv, j=j: o.copy(out=hT[:, :, j * 128:(j + 1) * 128], in_=pv),
                     reads=[("ps", b0), ("ps", b0 + 1)], writes=[("hT", j)])
                S.op("dve", lambda o, pv=pv, s2=s2: o.tensor_copy(out=hTf[s2], in_=pv),
                     reads=[("ps", b0), ("ps", b0 + 1)], writes=[("hTf", s2)])
                lb = 4 + s2

                def lgm(o, s2=s2, lb=lb):
                    r = None
                    for c in range(KC):
                        r = o.matmul(ps[:, lb, 0:NE], lhsT=hTf[s2][:, c, :], rhs=rw[:, c, :],
                                     start=(c == 0), stop=(c == KC - 1))
                    return r
                S.op("pe", lgm, reads=[("hTf", s2), "rw"], writes=[("ps", lb)])
                S.op("dve", lambda o, lb=lb, j=j: o.tensor_copy(out=lg[:, j, :], in_=ps[:, lb, 0:NE]),
                     reads=[("ps", lb)], writes=[("lg", j)])
                S.op("dve", lambda o, j=j: o.max(out=m8[:, j, :], in_=lg[:, j, :]), reads=[("lg", j)], writes=[("m8", j)])
                S.op("dve", lambda o, j=j: o.tensor_tensor(out=wgt[:, j, 0:1], in0=m8[:, j, 0:1], in1=m8[:, j, 1:2],
                                                           op=ALU.subtract), reads=[("m8", j)], writes=[("wg0", j)])
                S.op("act", lambda o, j=j: o.activation(out=wgt[:, j, 1:2], in_=wgt[:, j, 0:1], func=AF.Sigmoid),
                     reads=[("wg0", j)], writes=[("wg1", j)])
                S.op("dve", lambda o, j=j: o.tensor_scalar(out=wgt[:, j, 2:3], in0=wgt[:, j, 1:2], scalar1=-1.0, scalar2=1.0,
                                                           op0=ALU.mult, op1=ALU.add), reads=[("wg1", j)], writes=[("wg2", j)])
                S.op("dve", lambda o, j=j: o.tensor_scalar(out=e1[:, j, :], in0=lg[:, j, :], scalar1=m8[:, j, 0:1],
                                                           scalar2=wgt[:, j, 1:2], op0=ALU.is_equal, op1=ALU.mult),
                     reads=[("lg", j), ("m8", j), ("wg1", j)], writes=[("e1", j)])
                S.op("dve", lambda o, j=j: o.tensor_scalar(out=comb[:, j, :], in0=lg[:, j, :], scalar1=m8[:, j, 1:2],
                                                           scalar2=wgt[:, j, 2:3], op0=ALU.is_equal, op1=ALU.mult),
                     reads=[("lg", j), ("m8", j), ("wg2", j)], writes=[("comb", j)])
                S.op("dve", lambda o, j=j: o.tensor_tensor(out=comb[:, j, :], in0=comb[:, j, :], in1=e1[:, j, :], op=ALU.add),
                     reads=[("comb", j), ("e1", j)], writes=[("comb", j)])

        tl = tiles_for(need_ctx)
        for step in range(len(tl) + 2):
            if step < len(tl):
                stA(tl[step])
            if 0 <= step - 1 < len(tl):
                stB(tl[step - 1])
            if 0 <= step - 2 < len(tl):
                stC(tl[step - 2])
        S.barrier()
        ar.release(m)

    def resid_update(j, c0, cw, psv, gate, tmp, key, comb_ap=None, extra_reads=()):
        if comb_ap is None:
            S.op("dve", lambda o: o.tensor_tensor(out=tmp, in0=psv, in1=gate[:, c0:c0 + cw], op=ALU.mult),
                 reads=list(extra_reads) + [("t", id(gate))], writes=[key])
        else:
            S.op("dve", lambda o: o.scalar_tensor_tensor(out=tmp, in0=psv, scalar=comb_ap, in1=gate[:, c0:c0 + cw],
                                                         op0=ALU.mult, op1=ALU.mult),
                 reads=list(extra_reads) + [("t", id(gate)), ("comb", j)], writes=[key])
        S.op("pool", lambda o: o.tensor_tensor(out=xl[:, j, c0:c0 + cw], in0=xl[:, j, c0:c0 + cw], in1=tmp, op=ALU.add),
             reads=[key, ("xl", j)], writes=[("xl", j)])

    def wout_phase(l, b, w_src, need_ctx):
        m = ar.mark()
        wo = ar.alloc([128, KC, D], BF16)
        gate = [ar.alloc([128, D], F32) for _ in range(2)]
        tmp = [ar.alloc([128, D], F32) for _ in range(2)]
        wv = w_src.rearrange("(k p) n -> p k n", p=128)
        for h in range(2):
            S.dma("pool", wo[:, :, h * 512:(h + 1) * 512], wv[:, :, h * 512:(h + 1) * 512], writes=[("wo", h)])
        load_mod(gate[0], l, b, 2)
        if need_ctx:
            load_mod(gate[1], l, 2, 2)
        for j in tiles_for(need_ctx):
            ci = 0 if j < NTL else 1
            s2 = j % 2
            b0 = 2 * s2

            def mm(o, j=j, b0=b0):
                r = None
                for h in range(2):
                    for c in range(KC):
                        r = o.matmul(ps[:, b0 + h, :], lhsT=hT[:, c, j * 128:(j + 1) * 128],
                                     rhs=wo[:, c, h * 512:(h + 1) * 512], start=(c == 0), stop=(c == KC - 1))
                return r
            S.op("pe", mm, reads=[("hT", j), ("wo", 0), ("wo", 1)], writes=[("ps", b0), ("ps", b0 + 1)])
            psv = ps[:, b0:b0 + 2, :].rearrange("p a n -> p (a n)")
            resid_update(j, 0, D, psv, gate[ci], tmp[s2], ("tmp", s2), extra_reads=[("ps", b0), ("ps", b0 + 1)])
        S.barrier()
        ar.release(m)

    def ffn_phase(l, b, need_ctx, moe):
        m = ar.mark()
        i = l // 2
        NF = 4
        NS = 2
        wg = [ar.alloc([128, KC, NF * 128], BF16) for _ in range(NS)]
        wu = [ar.alloc([128, KC, NF * 128], BF16) for _ in range(NS)]
        wd = [ar.alloc([128, NF, D], BF16) for _ in range(NS)]
        gate = [ar.alloc([128, D], F32) for _ in range(2)]
        sg = [ar.alloc([128, 512], F32) for _ in range(2)]
        actT = [[ar.alloc([128, 512], BF16) for _ in range(NF)] for _ in range(2)]
        tmp = [ar.alloc([128, 512], F32) for _ in range(4)]
        load_mod(gate[0], l, b, 5)
        if need_ctx:
            load_mod(gate[1], l, 2, 5)
        blocks = blocks_for(need_ctx)
        ngroups = NFC // NF
        experts = list(range(NE)) if moe else [None]
        items = [(e, g) for e in experts for g in range(ngroups)]

        def load(idx):
            e, g = items[idx]
            s = idx % NS
            if moe:
                g_src, u_src, d_src = odwg_d[i, e], odwu_d[i, e], odwd_d[i, e]
            else:
                g_src, u_src, d_src = evwg_d[i], evwu_d[i], evwd_d[i]
            f0 = g * NF * 128
            S.dma("pool", wg[s], g_src.rearrange("(k p) f -> p k f", p=128)[:, :, f0:f0 + NF * 128], writes=[("wg", s)])
            S.dma("pool", wu[s], u_src.rearrange("(k p) f -> p k f", p=128)[:, :, f0:f0 + NF * 128], writes=[("wu", s)])
            S.dma("pool", wd[s], d_src[f0:f0 + NF * 128, :].rearrange("(f p) d -> p f d", p=128), writes=[("wd", s)])

        for idx in range(min(NS - 1, len(items))):
            load(idx)
        cnt = [0]
        pend = [None]

        def phaseB(args):
            e, s, t0, tw, aset = args
            for qt in range(tw // 128):
                j = (t0 // 128) + qt
                ci = 0 if j < NTL else 1
                for h in range(2):
                    k = cnt[0] % 4
                    cnt[0] += 1
                    bank = 4 + k

                    def mm(o, qt=qt, h=h, bank=bank, s=s, aset=aset):
                        r = None
                        for fi in range(NF):
                            r = o.matmul(ps[:, bank, :], lhsT=actT[aset][fi][:, qt * 128:(qt + 1) * 128],
                                         rhs=wd[s][:, fi, h * 512:(h + 1) * 512], start=(fi == 0), stop=(fi == NF - 1))
                        return r
                    S.op("pe", mm, reads=[("actT", aset, fi) for fi in range(NF)] + [("wd", s)], writes=[("ps", bank)])
                    cap = comb[:, j, e:e + 1] if moe else None
                    resid_update(j, h * 512, 512, ps[:, bank, :], gate[ci], tmp[k], ("tmp", k), comb_ap=cap,
                                 extra_reads=[("ps", bank)])

        ab = 0
        for idx, (e, g) in enumerate(items):
            if pend[0] is not None:
                phaseB(pend[0])
                pend[0] = None
            if idx + NS - 1 < len(items):
                load(idx + NS - 1)
            s = idx % NS
            for (t0, tw) in blocks:
                aset = ab % 2
                ab += 1
                for fi in range(NF):
                    pb = 2 * (fi % 2)

                    def mmA(o, fi=fi, pb=pb, s=s, t0=t0, tw=tw):
                        r = None
                        for (wsrc, bk) in ((wg[s], pb), (wu[s], pb + 1)):
                            for c in range(KC):
                                r = o.matmul(ps[:, bk, 0:tw], lhsT=wsrc[:, c, fi * 128:(fi + 1) * 128],
                                             rhs=hT[:, c, t0:t0 + tw], start=(c == 0), stop=(c == KC - 1))
                        return r
                    S.op("pe", mmA, reads=[("wg", s), ("wu", s)] + [("hT", jj) for jj in range(t0 // 128, (t0 + tw) // 128)],
                         writes=[("ps", pb), ("ps", pb + 1)])
                    s2 = fi % 2
                    S.op("act", lambda o, pb=pb, s2=s2, tw=tw: o.activation(out=sg[s2][:, 0:tw], in_=ps[:, pb, 0:tw], func=AF.Silu),
                         reads=[("ps", pb)], writes=[("sg", s2)])
                    S.op("dve", lambda o, pb=pb, s2=s2, tw=tw, aset=aset, fi=fi: o.tensor_tensor(
                        out=actT[aset][fi][:, 0:tw], in0=sg[s2][:, 0:tw], in1=ps[:, pb + 1, 0:tw], op=ALU.mult),
                        reads=[("sg", s2), ("ps", pb + 1)], writes=[("actT", aset, fi)])
                if pend[0] is not None:
                    phaseB(pend[0])
                pend[0] = (e, s, t0, tw, aset)
        if pend[0] is not None:
            phaseB(pend[0])
        S.barrier()
        ar.release(m)

    def even_mixer(l, b, need_ctx):
        i = l // 2
        m = ar.mark()
        QK = ar.alloc([128, 5, T], BF16)
        V = ar.alloc([128, NT, 2, HD + 1], BF16)
        yT = ar.alloc([128, 4, YT_W], BF16)
        cwT = ar.alloc([128, 4, 33], F32)
        m_attn = ar.mark()
        w_in = ar.alloc([128, KC, 768], BF16)
        gq = ar.alloc([128, 10, HD], F32)
        gsrc = ar.alloc([128, 2, HD], F32)
        sq = [ar.alloc([128, 640], F32) for _ in range(2)]
        qk = [ar.alloc([128, 640], F32) for _ in range(2)]
        rt = [ar.alloc([128, 320], F32) for _ in range(4)]
        qkb = [ar.alloc([128, 640], BF16) for _ in range(2)]
        st2 = ar.alloc([128, NT, 10], F32)
        cw33 = ar.alloc([128, 512], F32)
        wv = evwin_d[i].rearrange("(k p) n -> p k n", p=128)
        for kv in range(2):
            for ii in range(4):
                S.dma("pool", w_in[:, :, (ii * 2 + kv) * 64:(ii * 2 + kv + 1) * 64],
                      wv[:, :, 1024 + (kv * 4 + ii) * 64:1024 + (kv * 4 + ii + 1) * 64], writes=[("win", 2 + kv, ii)])
        S.dma("pool", w_in[:, :, 512:768], wv[:, :, 1536:1792], writes=[("win", 4)])
        S.dma("sp", gsrc[:, 0, :], evqg_d[i].partition_broadcast(128), writes=["gsrc0"])
        S.dma("sp", gsrc[:, 1, :], evkg_d[i].partition_broadcast(128), writes=["gsrc1"])
        S.op("dve", lambda o: o.tensor_copy(out=gq[:, 0:8, :], in_=gsrc[:, 0:1, :].broadcast_to([128, 8, HD])), reads=["gsrc0"], writes=["gq"])
        S.op("dve", lambda o: o.tensor_copy(out=gq[:, 8:10, :], in_=gsrc[:, 1:2, :].broadcast_to([128, 2, HD])), reads=["gsrc1"], writes=["gq2"])
        S.dma("sp", cw33[0:31, :], evcw_d[i], writes=["cw33a"])
        S.dma("sp", cw33[31:32, :], evlng_d[i].unsqueeze(0), writes=["cw33b"])
        S.dma("sp", cw33[32:33, :], evlnb_d[i].unsqueeze(0), writes=["cw33c"])
        S.op("dve", lambda o: o.memset(V, 1.0), writes=["V"])
        S.op("dve", lambda o: o.memset(yT, 0.0), writes=["yT"])

        def trc(o):
            r = None
            for cc in range(4):
                r = o.transpose(out=ps[:, 7, cc * 64:cc * 64 + 33], in_=cw33[0:33, cc * 128:(cc + 1) * 128],
                                identity=idf[0:33, 0:33])
            return r
        S.op("pe", trc, reads=["cw33a", "cw33b", "cw33c"], writes=[("ps", 7)])
        S.op("dve", lambda o: o.tensor_copy(out=cwT, in_=ps[:, 7, 0:256].rearrange("p (c w) -> p c w", c=4)[:, :, 0:33]),
             reads=[("ps", 7)], writes=["cwT"])
        S.barrier()
        def p1(j):
            s2 = j % 2
            b0 = 2 * s2
            lat = j < NTL

            def mm(o, j=j, b0=b0):
                r = None
                for (bk, c0, cw_) in ((b0, 0, 512), (b0 + 1, 512, 256)):
                    for c in range(KC):
                        r = o.matmul(ps[:, bk, 0:cw_], lhsT=hT[:, c, j * 128:(j + 1) * 128], rhs=w_in[:, c, c0:c0 + cw_],
                                     start=(c == 0), stop=(c == KC - 1))
                return r
            S.op("pe", mm, reads=[("hT", j), ("win", 4)] + [("win", 2 + kv_, i_) for kv_ in range(2) for i_ in range(4)], writes=[("ps", b0), ("ps", b0 + 1)])
            pq = ps[:, b0:b0 + 2, :].rearrange("p a n -> p (a n)")[:, 0:640]
            S.op("act", lambda o, pq=pq, s2=s2: o.activation(out=sq[s2], in_=pq, func=AF.Square),
                 reads=[("ps", b0), ("ps", b0 + 1)], writes=[("sq", s2)])
            S.op("dve", lambda o, s2=s2, j=j: o.tensor_reduce(out=st2[:, j, :], in_=sq[s2].rearrange("p (h d) -> p h d", h=10),
                                                              axis=AX.X, op=ALU.add), reads=[("sq", s2)], writes=[("st2", j)])
            S.op("act", lambda o, j=j: o.activation(out=st2[:, j, :], in_=st2[:, j, :], func=AF.Sqrt, bias=1e-6, scale=1.0 / HD),
                 reads=[("st2", j)], writes=[("st2", j)])
            S.op("dve", lambda o, j=j: o.reciprocal(out=st2[:, j, :], in_=st2[:, j, :]), reads=[("st2", j)], writes=[("st2", j)])
            qk3 = qk[s2].rearrange("p (h d) -> p h d", h=10)
            S.op("dve", lambda o, pq=pq, qk3=qk3, j=j: o.tensor_tensor(
                out=qk3, in0=pq.rearrange("p (h d) -> p h d", h=10), in1=st2[:, j, :].unsqueeze(2).broadcast_to([128, 10, HD]),
                op=ALU.mult), reads=[("ps", b0), ("ps", b0 + 1), ("st2", j)], writes=[("qk", s2)])
            S.op("dve", lambda o, qk3=qk3: o.tensor_tensor(out=qk3, in0=qk3, in1=gq, op=ALU.mult),
                 reads=[("qk", s2), "gq", "gq2"], writes=[("qk", s2)])
            if lat:
                rope(qk[s2], qkb[s2], 10, j, rt, ("qk", s2), ("qkb", s2))
            else:
                S.op("dve", lambda o, s2=s2: o.tensor_copy(out=qkb[s2], in_=qk[s2]), reads=[("qk", s2)], writes=[("qkb", s2)])
            S.op("act", lambda o, b0=b0, j=j: o.copy(out=V[:, j, :, 0:HD], in_=ps[:, b0 + 1, 128:256].rearrange("p (a d) -> p a d", a=2)),
                 reads=[("ps", b0 + 1)], writes=[("V", j)])
        def p2(j):
            s2 = j % 2
            lat = j < NTL
            do_q = lat or need_ctx
            tb = 4 + s2
            pv = psb16(tb)[:, 0:640].rearrange("p (c t) -> p c t", c=5)

            def tr(o, s2=s2, pv=pv, do_q=do_q):
                r = None
                for c in (range(5) if do_q else [4]):
                    r = o.transpose(out=pv[:, c, :], in_=qkb[s2][:, c * 128:(c + 1) * 128], identity=idb)
                return r
            S.op("pe", tr, reads=[("qkb", s2)], writes=[("ps", tb)])
            if do_q:
                S.op("act", lambda o, pv=pv, j=j: o.copy(out=QK[:, :, j * 128:(j + 1) * 128], in_=pv),
                     reads=[("ps", tb)], writes=[("QK", j)])
            else:
                S.op("act", lambda o, pv=pv, j=j: o.copy(out=QK[:, 4, j * 128:(j + 1) * 128], in_=pv[:, 4, :]),
                     reads=[("ps", tb)], writes=[("QK", j)])
        for k in range(NT + 1):
            if k < NT:
                p1(k)
            if k >= 1:
                p2(k - 1)
        S.barrier()
        ar.release(m_attn)
        w_ag = ar.alloc([128, KC, 1024], BF16)
        sgm = [ar.alloc([128, 512], F32) for _ in range(2)]
        S.dma("pool", w_ag[:, :, 0:512], wv[:, :, 0:512], writes=[("win", 0)])
        S.dma("pool", w_ag[:, :, 512:1024], wv[:, :, 512:1024], writes=[("win", 1)])
        k_ = 0
        for (t0, tw) in blocks_for(need_ctx):
            yoff = (PAD + t0) if t0 < TL else (CTX_OFF + t0 - TL)
            for cc in range(4):
                pb = 2 * (k_ % 2)
                s2 = k_ % 2
                k_ += 1

                def mm(o, cc=cc, pb=pb, t0=t0, tw=tw):
                    r = None
                    for (bk, c0) in ((pb, cc * 128), (pb + 1, 512 + cc * 128)):
                        for c in range(KC):
                            r = o.matmul(ps[:, bk, 0:tw], lhsT=w_ag[:, c, c0:c0 + 128], rhs=hT[:, c, t0:t0 + tw],
                                         start=(c == 0), stop=(c == KC - 1))
                    return r
                S.op("pe", mm, reads=[("win", 0), ("win", 1)] + [("hT", jj) for jj in range(t0 // 128, (t0 + tw) // 128)],
                     writes=[("ps", pb), ("ps", pb + 1)])
                S.op("act", lambda o, pb=pb, s2=s2, tw=tw: o.activation(out=sgm[s2][:, 0:tw], in_=ps[:, pb + 1, 0:tw], func=AF.Sigmoid),
                     reads=[("ps", pb + 1)], writes=[("sgm", s2)])
                S.op("dve", lambda o, pb=pb, s2=s2, tw=tw, cc=cc, yoff=yoff: o.tensor_tensor(
                    out=yT[:, cc, yoff:yoff + tw], in0=sgm[s2][:, 0:tw], in1=ps[:, pb, 0:tw], op=ALU.mult),
                    reads=[("sgm", s2), ("ps", pb)], writes=["yT"])
        S.barrier()
        ar.release(m_attn)
        PT = [ar.alloc([128, 512], BF16) for _ in range(4)]
        att = [ar.alloc([128, 4, 512], BF16) for _ in range(2)]
        rl = [ar.alloc([128, 4], F32) for _ in range(2)]
        convF = ar.alloc([128, 4, 512], F32)
        sqF = ar.alloc([128, 4, 512], F32)
        mean = ar.alloc([128, 512], F32)
        var = ar.alloc([128, 512], F32)
        z = [ar.alloc([128, 512], F32) for _ in range(2)]
        ptmp = [ar.alloc([128, 512], F32) for _ in range(2)]

        def conv_gen():
            for (t0, tw) in blocks_for(need_ctx):
                yoff = (PAD + t0) if t0 < TL else (CTX_OFF + t0 - TL)
                for tp in range(CONVW):
                    for cc in range(4):
                        eng = "dve"
                        if tp == 0:
                            S.op(eng, lambda o, cc=cc, yoff=yoff, tw=tw: o.tensor_scalar(
                                out=convF[:, cc, 0:tw], in0=yT[:, cc, yoff - PAD:yoff - PAD + tw], scalar1=cwT[:, cc, 0:1], scalar2=None,
                                op0=ALU.mult), reads=["yT", "cwT"], writes=[("convF", cc)])
                        elif cc < 4:
                            S.op(eng, lambda o, cc=cc, yoff=yoff, tw=tw, tp=tp: o.scalar_tensor_tensor(
                                out=convF[:, cc, 0:tw], in0=yT[:, cc, yoff - PAD + tp:yoff - PAD + tp + tw], scalar=cwT[:, cc, tp:tp + 1],
                                in1=convF[:, cc, 0:tw], op0=ALU.mult, op1=ALU.add), reads=[("convF", cc)], writes=[("convF", cc)])
                        else:
                            k2 = tp % 2
                            S.op(eng, lambda o, cc=cc, yoff=yoff, tw=tw, tp=tp, k2=k2: o.tensor_scalar(
                                out=ptmp[k2][:, 0:tw], in0=yT[:, cc, yoff - PAD + tp:yoff - PAD + tp + tw], scalar1=cwT[:, cc, tp:tp + 1],
                                scalar2=None, op0=ALU.mult), reads=["yT", "cwT"], writes=[("ptmp", k2)])
                            S.op(eng, lambda o, cc=cc, tw=tw, k2=k2: o.tensor_tensor(
                                out=convF[:, cc, 0:tw], in0=convF[:, cc, 0:tw], in1=ptmp[k2][:, 0:tw], op=ALU.add),
                                reads=[("convF", cc), ("ptmp", k2)], writes=[("convF", cc)])
                    yield
                S.op("act", lambda o, tw=tw: o.activation(out=sqF[:, :, 0:tw], in_=convF[:, :, 0:tw], func=AF.Square),
                     reads=[("convF", cc) for cc in range(4)], writes=["sqF"])

                def mst(o, tw=tw):
                    r = None
                    for (bk, src) in ((4, convF), (5, sqF)):
                        for cc in range(4):
                            r = o.matmul(ps[:, bk, 0:tw], lhsT=onesN, rhs=src[:, cc, 0:tw], start=(cc == 0), stop=(cc == 3))
                    return r
                S.op("pe", mst, reads=[("convF", cc) for cc in range(4)] + ["sqF", "onesN"], writes=[("ps", 4), ("ps", 5)])
                S.op("act", lambda o, tw=tw: o.copy(out=mean[:, 0:tw], in_=ps[:, 4, 0:tw]), reads=[("ps", 4)], writes=["mean"])
                S.op("dve", lambda o, tw=tw: o.tensor_tensor(out=var[:, 0:tw], in0=mean[:, 0:tw], in1=mean[:, 0:tw], op=ALU.mult),
                     reads=["mean"], writes=["var"])
                S.op("dve", lambda o, tw=tw: o.tensor_tensor(out=var[:, 0:tw], in0=ps[:, 5, 0:tw], in1=var[:, 0:tw], op=ALU.subtract),
                     reads=["var", ("ps", 5)], writes=["var"])
                S.op("act", lambda o, tw=tw: o.activation(out=var[:, 0:tw], in_=var[:, 0:tw], func=AF.Sqrt, bias=1e-5, scale=1.0),
                     reads=["var"], writes=["var"])
                S.op("dve", lambda o, tw=tw: o.reciprocal(out=var[:, 0:tw], in_=var[:, 0:tw]), reads=["var"], writes=["var"])
                yield
                for cc in range(4):
                    s2 = cc % 2
                    S.op("dve", lambda o, cc=cc, s2=s2, tw=tw: o.tensor_tensor(out=z[s2][:, 0:tw], in0=convF[:, cc, 0:tw], in1=mean[:, 0:tw],
                                                                             op=ALU.subtract), reads=[("convF", cc), "mean"], writes=[("z", s2)])
                    S.op("dve", lambda o, s2=s2, tw=tw: o.tensor_tensor(out=z[s2][:, 0:tw], in0=z[s2][:, 0:tw], in1=var[:, 0:tw], op=ALU.mult),
                         reads=[("z", s2), "var"], writes=[("z", s2)])
                    S.op("act", lambda o, cc=cc, s2=s2, tw=tw, t0=t0: o.activation(
                        out=hT[:, cc, t0:t0 + tw], in_=z[s2][:, 0:tw], func=AF.Silu, bias=cwT[:, cc, 32:33], scale=cwT[:, cc, 31:32]),
                        reads=[("z", s2), "cwT"], writes=[("hTc", cc, t0)])
                    yield

        ga = attention_gqa(QK, V, PT, att, rl, need_ctx)
        gc = conv_gen()
        a_alive = c_alive = True
        while a_alive or c_alive:
            if a_alive:
                try:
                    next(ga)
                except StopIteration:
                    a_alive = False
            if c_alive:
                try:
                    next(gc)
                except StopIteration:
                    c_alive = False
        S.barrier()
        ar.release(m)
        wout_phase(l, b, evwout_d[i], need_ctx)

    def rope(src, dst, nh, j, rt, rkey, wkey):
        sv = src.rearrange("p (h a t f) -> p h a t f", h=nh, a=2, t=2)
        dv = dst.rearrange("p (h a t f) -> p h a t f", h=nh, a=2, t=2)
        x1, x2 = sv[:, :, :, 0, :], sv[:, :, :, 1, :]
        cb = cos_t[:, j, :].rearrange("p (a f) -> p a f", a=2).unsqueeze(1).broadcast_to([128, nh, 2, 16])
        sb = sin_t[:, j, :].rearrange("p (a f) -> p a f", a=2).unsqueeze(1).broadcast_to([128, nh, 2, 16])
        n = nh * 32
        r = [t[:, 0:n].rearrange("p (h a f) -> p h a f", h=nh, a=2) for t in rt]
        S.op("dve", lambda o: o.tensor_tensor(out=r[0], in0=x1, in1=cb, op=ALU.mult), reads=[rkey, "cos"], writes=[("rt", 0)])
        S.op("pool", lambda o: o.tensor_tensor(out=r[1], in0=x2, in1=sb, op=ALU.mult), reads=[rkey, "sin"], writes=[("rt", 1)])
        S.op("dve", lambda o: o.tensor_tensor(out=r[2], in0=x2, in1=cb, op=ALU.mult), reads=[rkey, "cos"], writes=[("rt", 2)])
        S.op("pool", lambda o: o.tensor_tensor(out=r[3], in0=x1, in1=sb, op=ALU.mult), reads=[rkey, "sin"], writes=[("rt", 3)])
        S.op("dve", lambda o: o.tensor_tensor(out=dv[:, :, :, 0, :], in0=r[0], in1=r[1], op=ALU.subtract),
             reads=[("rt", 0), ("rt", 1)], writes=[(wkey, 0)])
        S.op("dve", lambda o: o.tensor_tensor(out=dv[:, :, :, 1, :], in0=r[2], in1=r[3], op=ALU.add),
             reads=[("rt", 2), ("rt", 3)], writes=[wkey])

    def attention_gqa(QK, V, PT, att, rl, need_ctx):
        qblocks = [(i * 512, 512, list(range(NT))) for i in range(4)]
        if need_ctx:
            qblocks.append((TL, TC, [16, 17]))
        step = [0]
        sctr = [0]
        for bi, (q0, qw, keys) in enumerate(qblocks):
            nqt = qw // 128
            a2 = bi % 2
            for h in range(8):
                kv, ii = h // 4, h % 4
                ob = 2 + (step[0] % 2)
                step[0] += 1
                p0 = kv * 64
                ov = ps[:, ob, :].rearrange("p (q n) -> p q n", q=4)

                def sT(o, s, sb_, p0=p0, ii=ii, q0=q0, qw=qw):
                    return o.matmul(ps[:, sb_, 0:qw], lhsT=QK[p0:p0 + 64, 4, s * 128:(s + 1) * 128],
                                    rhs=QK[p0:p0 + 64, ii, q0:q0 + qw], start=True, stop=True)

                def pvm(o, s, pt, first, last, kv=kv, ov=ov, nqt=nqt):
                    r = None
                    for qt in range(nqt):
                        r = o.matmul(ov[:, qt, 0:HD + 1], lhsT=pt[:, qt * 128:(qt + 1) * 128], rhs=V[:, s, kv, :],
                                     start=(first and qt == 0), stop=last, skip_group_check=True)
                    return r
                nk = len(keys)
                LA = 2
                SB = (0, 1, 6, 7)
                pts = []
                for si in range(nk + LA):
                    yield
                    if si < nk:
                        s = keys[si]
                        sb_ = SB[sctr[0] % 4]
                        pi = sctr[0] % 4
                        sctr[0] += 1
                        pts.append(pi)
                        S.op("pe", lambda o, s=s, sb_=sb_, sT=sT: sT(o, s, sb_),
                             reads=[("QK", s)] + [("QK", jj) for jj in range(q0 // 128, (q0 + qw) // 128)], writes=[("ps", sb_)])
                        pt = PT[pi]
                        S.op("act", lambda o, sb_=sb_, pt=pt, qw=qw: o.activation(out=pt[:, 0:qw], in_=ps[:, sb_, 0:qw], func=AF.Exp,
                                                                               scale=HD ** -0.5),
                             reads=[("ps", sb_)], writes=[("PT", pi)])
                    if si >= LA:
                        sp_ = keys[si - LA]
                        pi2 = pts[si - LA]
                        pt = PT[pi2]
                        S.op("pe", lambda o, sp_=sp_, pt=pt, first=(si == LA), last=(si == nk + LA - 1), pvm=pvm: pvm(o, sp_, pt, first, last),
                             reads=[("PT", pi2), ("V", sp_)], writes=[("ps", ob)])
                S.op("dve", lambda o, ov=ov, a2=a2, nqt=nqt: o.reciprocal(out=rl[a2][:, 0:nqt], in_=ov[:, 0:nqt, HD]),
                     reads=[("ps", ob)], writes=[("rl", a2)])
                S.op("dve", lambda o, ov=ov, a2=a2, nqt=nqt, h=h: o.tensor_tensor(
                    out=att[a2][:, 0:nqt, h * HD:(h + 1) * HD], in0=ov[:, 0:nqt, 0:HD],
                    in1=rl[a2][:, 0:nqt].unsqueeze(2).broadcast_to([128, nqt, HD]), op=ALU.mult),
                    reads=[("ps", ob), ("rl", a2)], writes=[("att", a2)])
            tb0 = 4
            pvv = ps[:, tb0:tb0 + 2, :].bitcast(BF16).rearrange("p a (c t) -> p (a c) t", c=2)[:, :, 0:qw]

            def tr(o, a2=a2, pvv=pvv, nqt=nqt):
                r = None
                for c in range(4):
                    for qt in range(nqt):
                        r = o.transpose(out=pvv[:, c, qt * 128:(qt + 1) * 128], in_=att[a2][:, qt, c * 128:(c + 1) * 128], identity=idb)
                return r
            S.op("pe", tr, reads=[("att", a2)], writes=[("ps", tb0), ("ps", tb0 + 1)])
            S.op("act", lambda o, pvv=pvv, q0=q0, qw=qw: o.copy(out=hT[:, 4:8, q0:q0 + qw], in_=pvv),
                 reads=[("ps", tb0), ("ps", tb0 + 1)], writes=[("hTa", q0)])

    def odd_mixer(l, b, need_ctx):
        i = l // 2
        lam_init = 0.8 - 0.6 * float(np.exp(-0.3 * l))
        m = ar.mark()
        mixT = ar.alloc([128, KC, T], BF16)
        m2 = ar.mark()
        wh = [ar.alloc([128, KC, 384], BF16) for _ in range(2)]
        QKh = [ar.alloc([128, 2, T], BF16) for _ in range(2)]
        Vh = [ar.alloc([128, NT, 129], BF16) for _ in range(2)]
        PT = [[ar.alloc([128, 512], BF16) for _ in range(2)] for _ in range(2)]
        qf = [ar.alloc([128, 256], F32) for _ in range(2)]
        qkb = [ar.alloc([128, 256], BF16) for _ in range(2)]
        rt = [ar.alloc([128, 128], F32) for _ in range(4)]
        lamt = ar.alloc([128, 4, HD], F32)
        lt = ar.alloc([128, 8], F32)
        sgb = ar.alloc([128, 128], F32)
        rl = ar.alloc([128, 4], F32)
        t0f = [ar.alloc([128, 2, 128], F32) for _ in range(2)]
        sqo = ar.alloc([128, 2, 128], F32)
        ob_ = [ar.alloc([128, 2, 128], BF16) for _ in range(2)]
        wv = odwin_d[i].rearrange("(k p) n -> p k n", p=128)

        def loadw(h):
            s = h % 2
            for part in range(3):
                S.dma("pool", wh[s][:, :, part * 128:(part + 1) * 128], wv[:, :, part * 1024 + h * 128:part * 1024 + (h + 1) * 128],
                      writes=[("wh", s, part)])
        loadw(0)
        S.dma("sp", lamt, odlam_d[i].partition_broadcast(128), writes=["lamt"])
        S.dma("sp", sgb, odsg_d[i].partition_broadcast(128), writes=["sgb"])
        S.op("dve", lambda o: o.memset(lt, 0.0), writes=["lt"])
        S.op("dve", lambda o: o.tensor_tensor(out=lamt[:, 0, :], in0=lamt[:, 0, :], in1=lamt[:, 1, :], op=ALU.mult), reads=["lamt"], writes=["lamt"])
        S.op("dve", lambda o: o.tensor_tensor(out=lamt[:, 2, :], in0=lamt[:, 2, :], in1=lamt[:, 3, :], op=ALU.mult), reads=["lamt"], writes=["lamt"])
        S.op("dve", lambda o: o.tensor_reduce(out=lt[:, 0:1], in_=lamt[:, 0, :], axis=AX.X, op=ALU.add), reads=["lamt"], writes=["lt"])
        S.op("dve", lambda o: o.tensor_reduce(out=lt[:, 1:2], in_=lamt[:, 2, :], axis=AX.X, op=ALU.add), reads=["lamt"], writes=["lt"])
        S.op("act", lambda o: o.activation(out=lt[:, 2:4], in_=lt[:, 0:2], func=AF.Exp), reads=["lt"], writes=["lt"])
        S.op("dve", lambda o: o.tensor_tensor(out=lt[:, 4:5], in0=lt[:, 3:4], in1=lt[:, 2:3], op=ALU.subtract), reads=["lt"], writes=["lt"])
        S.op("dve", lambda o: o.tensor_scalar(out=lt[:, 5:6], in0=lt[:, 4:5], scalar1=-lam_init, scalar2=None, op0=ALU.add), reads=["lt"], writes=["lt"])
        S.op("dve", lambda o: o.tensor_scalar(out=sgb, in0=sgb, scalar1=(1.0 - lam_init), scalar2=None, op0=ALU.mult), reads=["sgb"], writes=["sgb"])
        S.op("dve", lambda o: o.memset(Vh[0], 1.0), writes=["Vh0"])
        S.op("dve", lambda o: o.memset(Vh[1], 1.0), writes=["Vh1"])
        S.barrier()
        neglam = lt[:, 5:6]
        qblocks = [(ii * 512, 512, list(range(NT))) for ii in range(4)]
        if need_ctx:
            qblocks.append((TL, TC, [16, 17]))
        def proj_gen(h):
            s = h % 2

            def pmm(j):
                lat = j < NTL
                s2 = j % 2

                def mm(o, j=j, s=s):
                    r = None
                    for c in range(KC):
                        r = o.matmul(ps[:, 6, 0:384], lhsT=hT[:, c, j * 128:(j + 1) * 128], rhs=wh[s][:, c, :],
                                     start=(c == 0), stop=(c == KC - 1))
                    return r
                S.op("pe", mm, reads=[("hT", j)] + [("wh", s, p_) for p_ in range(3)], writes=[("ps", 6)])
                S.op("act", lambda o, j=j: o.copy(out=Vh[s][:, j, 0:128], in_=ps[:, 6, 256:384]), reads=[("ps", 6)], writes=[("Vh", s, j)])
                if lat:
                    S.op("act", lambda o, s2=s2: o.copy(out=qf[s2], in_=ps[:, 6, 0:256]), reads=[("ps", 6)], writes=[("qf", s2)])
                    rope(qf[s2], qkb[s2], 4, j, rt, ("qf", s2), ("qkb", s2))
                else:
                    S.op("act", lambda o, s2=s2: o.copy(out=qkb[s2], in_=ps[:, 6, 0:256]), reads=[("ps", 6)], writes=[("qkb", s2)])

            def ptr(j):
                lat = j < NTL
                do_q = lat or need_ctx
                s2 = j % 2
                pv = psb16(7)[:, 0:256].rearrange("p (c t) -> p c t", c=2)

                def tr(o, s2=s2, pv=pv, do_q=do_q):
                    r = None
                    for c in (range(2) if do_q else [1]):
                        r = o.transpose(out=pv[:, c, :], in_=qkb[s2][:, c * 128:(c + 1) * 128], identity=idb)
                    return r
                S.op("pe", tr, reads=[("qkb", s2)], writes=[("ps", 7)])
                if do_q:
                    S.op("act", lambda o, pv=pv, j=j: o.copy(out=QKh[s][:, :, j * 128:(j + 1) * 128], in_=pv), reads=[("ps", 7)], writes=[("QKh", s, j)])
                else:
                    S.op("act", lambda o, pv=pv, j=j: o.copy(out=QKh[s][:, 1, j * 128:(j + 1) * 128], in_=pv[:, 1, :]), reads=[("ps", 7)], writes=[("QKh", s, j)])
            for k in range(NT + 1):
                if k < NT:
                    pmm(k)
                if k >= 1:
                    ptr(k - 1)
                yield

        def attn_gen(h):
            s = h % 2
            for (q0, qw, keys) in qblocks:
                nqt = qw // 128
                nk = len(keys)
                def ovw(c, qt):
                    return ps[:, 2 + 2 * c + qt // 2, :][:, (qt % 2) * 256:(qt % 2) * 256 + 129]
                for si in range(nk + 1):
                    if si == min(3, nk) and deferred:
                        for f_ in deferred:
                            f_()
                        del deferred[:]
                    if si < nk:
                        sk = keys[si]
                        for c in range(2):
                            sbk = (c, 6 + c)[si % 2]
                            S.op("pe", lambda o, sk=sk, c=c, q0=q0, qw=qw, sbk=sbk: o.matmul(
                                ps[:, sbk, 0:qw], lhsT=QKh[s][c * 64:(c + 1) * 64, 1, sk * 128:(sk + 1) * 128],
                                rhs=QKh[s][c * 64:(c + 1) * 64, 0, q0:q0 + qw], start=True, stop=True),
                                reads=[("QKh", s, sk)] + [("QKh", s, jj) for jj in range(q0 // 128, (q0 + qw) // 128)], writes=[("ps", sbk)])
                            pt = PT[c][si % 2]
                            S.op("act", lambda o, c=c, pt=pt, qw=qw, sbk=sbk: o.activation(out=pt[:, 0:qw], in_=ps[:, sbk, 0:qw], func=AF.Exp, scale=HD ** -0.5),
                                 reads=[("ps", sbk)], writes=[("PT", c, si % 2)])
                    if si >= 1:
                        sp_ = keys[si - 1]
                        for c in range(2):
                            pt = PT[c][(si - 1) % 2]

                            def pvm(o, c=c, pt=pt, sp_=sp_, first=(si == 1), last=(si == nk), nqt=nqt, ovw=ovw):
                                r = None
                                for qt in range(nqt):
                                    r = o.matmul(ovw(c, qt), lhsT=pt[:, qt * 128:(qt + 1) * 128], rhs=Vh[s][:, sp_, :],
                                                 start=(first and qt % 2 == 0), stop=last, skip_group_check=True)
                                return r
                            S.op("pe", pvm, reads=[("PT", c, (si - 1) % 2), ("Vh", s, sp_)], writes=[("ps", 2 + 2 * c), ("ps", 3 + 2 * c)])
                for hf in range(nqt // 2):
                    o0 = ps[:, 2 + hf, :].rearrange("p (q n) -> p q n", q=2)
                    o1 = ps[:, 4 + hf, :].rearrange("p (q n) -> p q n", q=2)
                    s2 = hf % 2
                    rd = [("ps", 2 + hf), ("ps", 4 + hf)]
                    S.op("dve", lambda o, o0=o0: o.reciprocal(out=rl[:, 0:2], in_=o0[:, :, 128]), reads=rd, writes=["rl0"])
                    S.op("dve", lambda o, o1=o1: o.reciprocal(out=rl[:, 2:4], in_=o1[:, :, 128]), reads=rd, writes=["rl1"])
                    S.op("dve", lambda o, o0=o0, s2=s2: o.tensor_tensor(out=t0f[s2], in0=o0[:, :, 0:128],
                                                                       in1=rl[:, 0:2].unsqueeze(2).broadcast_to([128, 2, 128]), op=ALU.mult),
                         reads=rd + ["rl0"], writes=[("t0f", s2)])
                    S.op("dve", lambda o, o1=o1: o.scalar_tensor_tensor(out=sqo, in0=o1[:, :, 0:128], scalar=neglam,
                                                                       in1=rl[:, 2:4].unsqueeze(2).broadcast_to([128, 2, 128]),
                                                                       op0=ALU.mult, op1=ALU.mult), reads=rd + ["rl1", "lt"], writes=["sqo"])
                    S.op("dve", lambda o, s2=s2: o.tensor_tensor(out=t0f[s2], in0=t0f[s2], in1=sqo, op=ALU.add),
                         reads=[("t0f", s2), "sqo"], writes=[("t0f", s2)])
                    S.op("act", lambda o, s2=s2: o.activation(out=sqo, in_=t0f[s2], func=AF.Square), reads=[("t0f", s2)], writes=["sqo"])
                    S.op("dve", lambda o: o.tensor_reduce(out=rl[:, 0:2], in_=sqo, axis=AX.X, op=ALU.add), reads=["sqo", "rl0"], writes=["rl0"])
                    S.op("act", lambda o: o.activation(out=rl[:, 0:2], in_=rl[:, 0:2], func=AF.Ln, bias=1e-6, scale=1.0 / 128), reads=["rl0"], writes=["rl0"])
                    S.op("act", lambda o: o.activation(out=rl[:, 0:2], in_=rl[:, 0:2], func=AF.Exp, scale=-0.5), reads=["rl0"], writes=["rl0"])
                    S.op("dve", lambda o, s2=s2: o.tensor_tensor(out=t0f[s2], in0=t0f[s2], in1=rl[:, 0:2].unsqueeze(2).broadcast_to([128, 2, 128]),
                                                                op=ALU.mult), reads=[("t0f", s2), "rl0"], writes=[("t0f", s2)])
                    S.op("dve", lambda o, s2=s2: o.tensor_tensor(out=ob_[s2], in0=t0f[s2], in1=sgb.unsqueeze(1).broadcast_to([128, 2, 128]),
                                                                op=ALU.mult), reads=[("t0f", s2), "sgb"], writes=[("ob", s2)])
                    pv = psb16(7)[:, 0:256].rearrange("p (c t) -> p c t", c=2)

                    def tr(o, s2=s2, pv=pv):
                        r = None
                        for qt in range(2):
                            r = o.transpose(out=pv[:, qt, :], in_=ob_[s2][:, qt, :], identity=idb)
                        return r
                    tq = q0 + hf * 256

                    def fin(tr=tr, s2=s2, pv=pv, tq=tq, h=h):
                        S.op("pe", tr, reads=[("ob", s2)], writes=[("ps", 7)])
                        S.op("act", lambda o, pv=pv, tq=tq, h=h: o.copy(out=mixT[:, h, tq:tq + 256], in_=pv.rearrange("p c t -> p (c t)")),
                             reads=[("ps", 7)], writes=[("mixT", h, tq)])
                    deferred.append(fin)
                yield

        deferred = []
        for _ in proj_gen(0):
            pass
        for h in range(8):
            pg = None
            if h + 1 < 8:
                loadw(h + 1)
                pg = proj_gen(h + 1)
            for _ in attn_gen(h):
                if pg is not None:
                    for _k in range(4):
                        try:
                            next(pg)
                        except StopIteration:
                            pg = None
                            break
            if pg is not None:
                for _ in pg:
                    pass
        for f_ in deferred:
            f_()
        del deferred[:]
        S.barrier()
        ar.release(m2)
        wo = ar.alloc([128, KC, D], BF16)
        gate = [ar.alloc([128, D], F32) for _ in range(2)]
        tmp = [ar.alloc([128, D], F32) for _ in range(2)]
        wvo = odwout_d[i].rearrange("(k p) n -> p k n", p=128)
        for hh in range(2):
            S.dma("pool", wo[:, :, hh * 512:(hh + 1) * 512], wvo[:, :, hh * 512:(hh + 1) * 512], writes=[("wo", hh)])
        load_mod(gate[0], l, b, 2)
        if need_ctx:
            load_mod(gate[1], l, 2, 2)
        for j in tiles_for(need_ctx):
            ci = 0 if j < NTL else 1
            s2 = j % 2
            b0 = 2 * s2

            def mm(o, j=j, b0=b0):
                r = None
                for hh in range(2):
                    for c in range(KC):
                        r = o.matmul(ps[:, b0 + hh, :], lhsT=mixT[:, c, j * 128:(j + 1) * 128],
                                     rhs=wo[:, c, hh * 512:(hh + 1) * 512], start=(c == 0), stop=(c == KC - 1))
                return r
            S.op("pe", mm, reads=[("wo", 0), ("wo", 1)], writes=[("ps", b0), ("ps", b0 + 1)])
            psv = ps[:, b0:b0 + 2, :].rearrange("p a n -> p (a n)")
            resid_update(j, 0, D, psv, gate[ci], tmp[s2], ("tmp", s2), extra_reads=[("ps", b0), ("ps", b0 + 1)])
        S.barrier()
        ar.release(m)

    def dump_xl(b):
        for j in range(NTL):
            S.dma("sp", out_d[b, j * 128:(j + 1) * 128, :], xl[:, j, :], reads=[("xl", j)])

    def final_phase(b):
        m = ar.mark()
        g = ar.alloc([128, D], F32)
        junk = ar.alloc([128, D], F32)
        o_t = [ar.alloc([128, D], F32) for _ in range(2)]
        S.dma("sp", g, fng_d.partition_broadcast(128), writes=["fg"])
        S.op("dve", lambda o: o.memset(stat, 0.0), writes=["stat"])
        for j in range(NTL):
            s2 = j % 2
            xj = xl[:, j, :]
            S.op("act", lambda o, xj=xj, j=j: o.activation(out=junk, in_=xj, func=AF.Square, accum_out=stat[:, j:j + 1]),
                 reads=[("xl", j), "stat"], writes=["junk", ("stat", j)])
            S.op("act", lambda o, j=j: o.activation(out=stat[:, 32 + j:33 + j], in_=stat[:, j:j + 1], func=AF.Sqrt, bias=1e-6, scale=1.0 / D),
                 reads=[("stat", j)], writes=[("stat2", j)])
            S.op("dve", lambda o, j=j: o.reciprocal(out=stat[:, 32 + j:33 + j], in_=stat[:, 32 + j:33 + j]), reads=[("stat2", j)], writes=[("stat2", j)])
            S.op("dve", lambda o, xj=xj, j=j, s2=s2: o.scalar_tensor_tensor(out=o_t[s2], in0=xj, scalar=stat[:, 32 + j:33 + j], in1=g,
                                                                           op0=ALU.mult, op1=ALU.mult),
                 reads=[("xl", j), ("stat2", j), "fg"], writes=[("ot", s2)])
            S.dma("sp", out_d[b, j * 128:(j + 1) * 128, :], o_t[s2], reads=[("ot", s2)])
        S.barrier()
        ar.release(m)

    for b in range(nb):
        for j in range(NTL):
            S.dma("sp", xl[:, j, :], x_d[b, j * 128:(j + 1) * 128, :], writes=[("xl", j)])
        for j in range(2):
            S.dma("sp", xl[:, NTL + j, :], ctx_d[b, j * 128:(j + 1) * 128, :], writes=[("xl", NTL + j)])
        S.barrier()
        done = False
        for l in range(n_layers):
            need_ctx = l < DEPTH - 1
            norm_phase(l, b, 1, True)
            if stop == ("norm1", l):
                done = True
                break
            if l % 2 == 0:
                even_mixer(l, b, need_ctx)
            else:
                odd_mixer(l, b, need_ctx)
            if stop == ("mix", l):
                done = True
                break
            if l % 2 == 0:
                norm_phase(l, b, 2, need_ctx)
                ffn_phase(l, b, need_ctx, moe=False)
            else:
                norm_phase(l, b, 2, need_ctx, router=l // 2)
                if stop == ("norm2", l):
                    done = True
                    break
                ffn_phase(l, b, need_ctx, moe=True)
        if stop is not None or n_layers < DEPTH:
            dump_xl(b)
        else:
            final_phase(b)
        S.barrier()
    S.finish("sp")
    S.emit()
    pcm.__exit__(None, None, None)
    ar.close()
    return nc


def _rope_tables():
    rows = TL // 64
    r = np.broadcast_to(np.arange(rows, dtype=np.float32)[:, None], (rows, 64)).reshape(-1)
    col = np.broadcast_to(np.arange(64, dtype=np.float32)[None, :], (rows, 64)).reshape(-1)
    inv = (np.float32(10000.0) ** (-np.arange(16, dtype=np.float32) / np.float32(16))).astype(np.float32)
    ang = np.stack([r[:, None] * inv, col[:, None] * inv], axis=1).astype(np.float32)
    return (np.cos(ang).reshape(TL, 32).astype(np.float32), np.sin(ang).reshape(TL, 32).astype(np.float32))


_WEIGHT_KEYS = ["c_ctx", "ada_w", "ada_b", "norm1_g", "norm2_g", "ev_w_in", "ev_conv_w", "ev_ln_g", "ev_ln_b",
                "ev_q_norm_g", "ev_k_norm_g", "ev_w_out", "ev_ffn_wg", "ev_ffn_wu", "ev_ffn_wd", "od_w_in", "od_lam",
                "od_subln_g", "od_w_out", "od_router_w", "od_moe_wg", "od_moe_wu", "od_moe_wd", "final_norm_g"]


def make_in_maps(inputs, cores):
    cos, sin = _rope_tables()
    maps = []
    shared = {k: np.ascontiguousarray(np.asarray(inputs[k], dtype=np.float32)) for k in _WEIGHT_KEYS}
    for c in cores:
        mp = dict(shared)
        mp["x"] = np.ascontiguousarray(np.asarray(inputs["x"][NB * c:NB * (c + 1)], dtype=np.float32))
        mp["c"] = np.ascontiguousarray(np.asarray(inputs["c"][NB * c:NB * (c + 1)], dtype=np.float32))
        mp["ctx"] = np.ascontiguousarray(np.asarray(inputs["ctx"][NB * c:NB * (c + 1)], dtype=np.float32))
        mp["rope_cos"] = cos
        mp["rope_sin"] = sin
        maps.append(mp)
    return maps


def kernel(**inputs):
    nc = build_program()
    cores = list(range(8))
    in_maps = make_in_maps(inputs, cores)
    res = run_bass_kernel_spmd(nc, in_maps, core_ids=cores)
    out = np.concatenate([np.asarray(r["out"], dtype=np.float32) for r in res.results], axis=0)
    return out
```

```python
import numpy as np
import concourse.bass as bass
import concourse.mybir as mybir
from concourse.bass_utils import run_bass_kernel_spmd

F32 = mybir.dt.float32
BF16 = mybir.dt.bfloat16
ALU = mybir.AluOpType
AF = mybir.ActivationFunctionType
AX = mybir.AxisListType
DT_SIZE = {F32: 4, BF16: 2}

D = 1024
KC = 8
TL = 2048
TC = 256
T = TL + TC
NT = T // 128
NTL = TL // 128
FF = 3584
NFC = FF // 128
NE = 8
HD = 64
DEPTH = 4
NB = 2
EVEN_IN = 1792
ODD_IN = 3072
CONVW = 31
PAD = 15
YT_W = PAD + TL + PAD + TC + PAD + 3
CTX_OFF = PAD + TL + PAD


class _Eng:
    def __init__(self, name, sem, unit):
        self.name = name
        self.sem = sem
        self.unit = unit
        self.count = 0
        self.thunks = []
        self.waited = {}


class Sched:
    REAL = ("pe", "act", "dve", "pool", "sp")

    def __init__(self, nc, n_dma_chan=28):
        self.nc = nc
        self.eng = {}
        self._sems = []
        for n in self.REAL:
            self.eng[n] = _Eng(n, self._new_sem("s_" + n), 1)
        self.chans = []
        for i in range(n_dma_chan):
            e = _Eng("dma%d" % i, self._new_sem("s_dma%d" % i), 16)
            self.eng[e.name] = e
            self.chans.append(e)
        self.next_chan = 0
        self.last_write = {}
        self.readers = {}

    def _new_sem(self, name):
        cm = self.nc.semaphore(name)
        s = cm.__enter__()
        self._sems.append(cm)
        return s

    def _deps(self, reads, writes):
        deps = {}

        def add(d):
            if d is None:
                return
            n, i = d
            if deps.get(n, 0) < i:
                deps[n] = i
        for r in reads:
            add(self.last_write.get(r))
        for w in writes:
            add(self.last_write.get(w))
            for n, i in self.readers.get(w, {}).items():
                add((n, i))
        return deps

    def _emit_waits(self, e, deps):
        for n, i in deps.items():
            if n == e.name and n == "pe":
                continue
            if e.waited.get(n, 0) >= i:
                continue
            e.waited[n] = i
            d = self.eng[n]
            val = i * d.unit
            sem = d.sem
            e.thunks.append(lambda o, sem=sem, val=val: o.wait_ge(sem, val))

    def _commit(self, name, idx, reads, writes):
        for r in reads:
            self.readers.setdefault(r, {})[name] = idx
        for w in writes:
            self.last_write[w] = (name, idx)
            self.readers[w] = {}

    def op(self, eng, fn, reads=(), writes=()):
        ps_r = [r for r in reads if isinstance(r, tuple) and r[0] == "ps"]
        if ps_r:
            writes = list(writes) + [r for r in ps_r if r not in writes]
        e = self.eng[eng]
        self._emit_waits(e, self._deps(reads, writes))
        e.count += 1
        sem = e.sem

        def thunk(o, fn=fn, sem=sem):
            ins = fn(o)
            ins.then_inc(sem, 1)
        e.thunks.append(thunk)
        self._commit(eng, e.count, reads, writes)

    def dma(self, queue, out, in_, reads=(), writes=(), **kw):
        q = self.eng[queue]
        ch = self.chans[self.next_chan]
        self.next_chan = (self.next_chan + 1) % len(self.chans)
        deps = self._deps(reads, writes)
        if ch.count > 0:
            deps[ch.name] = max(deps.get(ch.name, 0), ch.count)
        self._emit_waits(q, deps)
        ch.count += 1
        sem = ch.sem
        q.thunks.append(lambda o, out=out, in_=in_, sem=sem, kw=kw:
                        o.dma_start(out=out, in_=in_, **kw).then_inc(sem, 16))
        self._commit(ch.name, ch.count, reads, writes)

    def barrier(self):
        for n in self.REAL:
            e = self.eng[n]
            deps = {m: d.count for m, d in self.eng.items() if d.count > 0 and m != n}
            self._emit_waits(e, deps)

    def finish(self, final_eng="sp"):
        e = self.eng[final_eng]
        deps = {m: d.count for m, d in self.eng.items() if d.count > 0 and m != final_eng}
        self._emit_waits(e, deps)

    def emit(self):
        nc = self.nc
        with nc.Block() as block:
            @block.tensor
            def _(o):
                for t in self.eng["pe"].thunks:
                    t(o)

            @block.scalar
            def _(o):
                for t in self.eng["act"].thunks:
                    t(o)

            @block.vector
            def _(o):
                for t in self.eng["dve"].thunks:
                    t(o)

            @block.gpsimd
            def _(o):
                for t in self.eng["pool"].thunks:
                    t(o)

            @block.sync
            def _(o):
                for t in self.eng["sp"].thunks:
                    t(o)
        for cm in reversed(self._sems):
            cm.__exit__(None, None, None)


class Arena:
    def __init__(self, nc, nbytes):
        self.words = nbytes // 4
        self.cm = nc.sbuf_tensor("arena", [128, self.words], F32)
        self.t = self.cm.__enter__()
        self.top = 0
        self.peak = 0

    def mark(self):
        return self.top

    def release(self, m):
        self.top = m

    def alloc(self, shape, dtype):
        assert shape[0] == 128
        n = int(np.prod(shape[1:]))
        nw = (n * DT_SIZE[dtype] + 3) // 4
        nw = (nw + 7) // 8 * 8
        off = self.top
        self.top += nw
        assert self.top <= self.words, "arena overflow %d > %d" % (self.top * 4, self.words * 4)
        self.peak = max(self.peak, self.top)
        v = self.t[:, off:off + nw]
        if dtype != F32:
            v = v.bitcast(dtype)
        v = v[:, 0:n]
        if len(shape) > 2:
            names = " ".join("d%d" % i for i in range(len(shape) - 1))
            kw = {"d%d" % i: shape[i + 1] for i in range(len(shape) - 1)}
            v = v.rearrange("p (%s) -> p %s" % (names, names), **kw)
        return v

    def close(self):
        self.cm.__exit__(None, None, None)


def bc(ap, shape):
    return ap.broadcast_to(shape)


def build_program(n_layers=DEPTH, nb=NB, stop=None):
    nc = bass.Bass("TRN2", target_bir_lowering=False)

    def din(name, shape):
        return nc.dram_tensor(name, list(shape), F32, kind="ExternalInput").ap()

    x_d = din("x", [NB, TL, D])
    c_d = din("c", [NB, D])
    ctx_d = din("ctx", [NB, TC, D])
    cctx_d = din("c_ctx", [D])
    adaw_d = din("ada_w", [DEPTH, D, 6 * D])
    adab_d = din("ada_b", [DEPTH, 6 * D])
    n1g_d = din("norm1_g", [DEPTH, D])
    n2g_d = din("norm2_g", [DEPTH, D])
    evwin_d = din("ev_w_in", [2, D, EVEN_IN])
    evcw_d = din("ev_conv_w", [2, CONVW, 512])
    evlng_d = din("ev_ln_g", [2, 512])
    evlnb_d = din("ev_ln_b", [2, 512])
    evqg_d = din("ev_q_norm_g", [2, HD])
    evkg_d = din("ev_k_norm_g", [2, HD])
    evwout_d = din("ev_w_out", [2, D, D])
    evwg_d = din("ev_ffn_wg", [2, D, FF])
    evwu_d = din("ev_ffn_wu", [2, D, FF])
    evwd_d = din("ev_ffn_wd", [2, FF, D])
    odwin_d = din("od_w_in", [2, D, ODD_IN])
    odlam_d = din("od_lam", [2, 4, HD])
    odsg_d = din("od_subln_g", [2, 128])
    odwout_d = din("od_w_out", [2, D, D])
    odrw_d = din("od_router_w", [2, D, NE])
    odwg_d = din("od_moe_wg", [2, NE, D, FF])
    odwu_d = din("od_moe_wu", [2, NE, D, FF])
    odwd_d = din("od_moe_wd", [2, NE, FF, D])
    fng_d = din("final_norm_g", [D])
    cos_d = din("rope_cos", [TL, 32])
    sin_d = din("rope_sin", [TL, 32])
    out_d = nc.dram_tensor("out", [NB, TL, D], F32, kind="ExternalOutput").ap()
    mods_d = nc.dram_tensor("mods", [DEPTH, 3, 6 * D], F32, kind="Internal").ap()

    S = Sched(nc)
    ar = Arena(nc, 206 * 1024)
    pcm = nc.psum_tensor("ps", [128, 8, 512], F32)
    ps = pcm.__enter__()

    def psb(b):
        return ps[:, b, :]

    def psb16(b):
        return ps[:, b, :].bitcast(BF16)

    xl = ar.alloc([128, NT, D], F32)
    hT = ar.alloc([128, KC, T], BF16)
    idb = ar.alloc([128, 128], BF16)
    idf = ar.alloc([128, 128], F32)
    onesN = ar.alloc([128, 128], F32)
    cos_t = ar.alloc([128, NTL, 32], F32)
    sin_t = ar.alloc([128, NTL, 32], F32)
    stat = ar.alloc([128, 64], F32)
    comb = ar.alloc([128, NT, NE], F32)
    base_mark = ar.mark()

    S.op("dve", lambda o: o.memset(idf, 0.0), writes=["idf"])
    S.op("pool", lambda o: o.affine_select(out=idf, in_=idf, pattern=[[-1, 128]], compare_op=ALU.not_equal,
                                           fill=1.0, base=0, channel_multiplier=1), reads=["idf"], writes=["idf"])
    S.op("dve", lambda o: o.tensor_copy(out=idb, in_=idf), reads=["idf"], writes=["idb"])
    S.op("dve", lambda o: o.memset(onesN, 1.0 / 512.0), writes=["onesN"])
    S.dma("sp", cos_t, cos_d.rearrange("(j p) f -> p j f", p=128), writes=["cos"])
    S.dma("sp", sin_t, sin_d.rearrange("(j p) f -> p j f", p=128), writes=["sin"])
    S.barrier()

    def prologue():
        m = ar.mark()
        c8 = ar.alloc([128, 3 * 128], F32)
        sT = ar.alloc([128, KC, 4], F32)
        wst = [ar.alloc([128, KC, 512], F32) for _ in range(2)]
        modrow = ar.alloc([128, 6 * D], F32)
        biasr = ar.alloc([128, 6 * D], F32)
        g1r = ar.alloc([128, D], F32)
        g2r = ar.alloc([128, D], F32)
        c8v = c8.rearrange("p (a b) -> p a b", a=3)
        for j in range(3):
            src = cctx_d if j == 2 else c_d[j]
            S.dma("sp", c8v[0:8, j, :], src.rearrange("(k p) -> k p", p=128), writes=["c8"])
        for j in range(3):
            S.op("pe", lambda o, j=j: o.transpose(out=ps[:, 0, j * 8:(j + 1) * 8], in_=c8v[0:8, j, :],
                                                  identity=idf[0:8, 0:8]), reads=["c8"], writes=[("ps", 0)])
        for j in range(3):
            S.op("act", lambda o, j=j: o.activation(out=sT[:, :, j], in_=ps[:, 0, j * 8:(j + 1) * 8], func=AF.Silu),
                 reads=[("ps", 0)], writes=["sT"])
        for l in range(DEPTH):
            S.dma("sp", biasr[0:3, :], adab_d[l].partition_broadcast(3), writes=["biasr"])
            S.dma("sp", g1r[0:3, :], n1g_d[l].partition_broadcast(3), writes=["g1r"])
            S.dma("sp", g2r[0:3, :], n2g_d[l].partition_broadcast(3), writes=["g2r"])
            wv = adaw_d[l].rearrange("(k p) n -> p k n", p=128)
            for nb_ in range(12):
                slot = nb_ % 2
                S.dma("sp", wst[slot], wv[:, :, nb_ * 512:(nb_ + 1) * 512], writes=[("wst", slot)])
                bank = 1 + slot

                def mm(o, slot=slot, bank=bank):
                    r = None
                    for k in range(KC):
                        r = o.matmul(ps[0:3, bank, :], lhsT=sT[:, k, 0:3], rhs=wst[slot][:, k, :],
                                     start=(k == 0), stop=(k == KC - 1))
                    return r
                S.op("pe", mm, reads=["sT", ("wst", slot)], writes=[("ps", bank)])
                S.op("dve", lambda o, bank=bank, nb_=nb_: o.tensor_tensor(
                    out=modrow[0:3, nb_ * 512:(nb_ + 1) * 512], in0=ps[0:3, bank, :],
                    in1=biasr[0:3, nb_ * 512:(nb_ + 1) * 512], op=ALU.add),
                    reads=[("ps", bank), "biasr"], writes=["modrow"])
            for ch, gr in ((1, g1r), (4, g2r)):
                S.op("dve", lambda o, ch=ch, gr=gr: o.scalar_tensor_tensor(
                    out=modrow[0:3, ch * D:(ch + 1) * D], in0=modrow[0:3, ch * D:(ch + 1) * D], scalar=1.0,
                    in1=gr[0:3, :], op0=ALU.add, op1=ALU.mult),
                    reads=["modrow", "g1r", "g2r"], writes=["modrow"])
            S.dma("sp", mods_d[l], modrow[0:3, :], reads=["modrow"], writes=["mods"])
        S.barrier()
        ar.release(m)

    prologue()

    def load_mod(dst, l, cond, chunk):
        S.dma("sp", dst, mods_d[l, cond, chunk * D:(chunk + 1) * D].partition_broadcast(128),
              reads=["mods"], writes=[("t", id(dst))])

    def tiles_for(need_ctx):
        return list(range(NT if need_ctx else NTL))

    def blocks_for(need_ctx):
        bl = [(i * 512, 512) for i in range(4)]
        if need_ctx:
            bl.append((TL, TC))
        return bl

    def norm_phase(l, b, which, need_ctx, router=None):
        m = ar.mark()
        ch_shift, ch_scale = (0, 1) if which == 1 else (3, 4)
        A = [ar.alloc([128, D], F32) for _ in range(2)]
        Sh = [ar.alloc([128, D], F32) for _ in range(2)]
        junk = ar.alloc([128, D], F32)
        t1 = [ar.alloc([128, D], F32) for _ in range(2)]
        nconds = 2 if need_ctx else 1
        for ci in range(nconds):
            cond = b if ci == 0 else 2
            load_mod(A[ci], l, cond, ch_scale)
            load_mod(Sh[ci], l, cond, ch_shift)
        if router is None:
            hb = [ar.alloc([128, D], BF16) for _ in range(2)]
        else:
            hb = [ar.alloc([128, D], F32) for _ in range(2)]
            hTf = [ar.alloc([128, KC, 128], F32) for _ in range(2)]
            rw = ar.alloc([128, KC, NE], F32)
            lg = ar.alloc([128, NT, NE], F32)
            m8 = ar.alloc([128, NT, 8], F32)
            wgt = ar.alloc([128, NT, 4], F32)
            e1 = ar.alloc([128, NT, NE], F32)
            S.dma("sp", rw, odrw_d[router].rearrange("(k p) e -> p k e", p=128), writes=["rw"])
        S.op("dve", lambda o: o.memset(stat, 0.0), writes=["stat"])
        def stA(j):
            xj = xl[:, j, :]
            S.op("act", lambda o, xj=xj, j=j: o.activation(out=junk, in_=xj, func=AF.Square, accum_out=stat[:, j:j + 1]),
                 reads=[("xl", j), "stat"], writes=["junk", ("stat", j)])
            S.op("act", lambda o, j=j: o.activation(out=stat[:, 32 + j:33 + j], in_=stat[:, j:j + 1], func=AF.Sqrt,
                                                    bias=1e-6, scale=1.0 / D), reads=[("stat", j)], writes=[("stat2", j)])
            S.op("dve", lambda o, j=j: o.reciprocal(out=stat[:, 32 + j:33 + j], in_=stat[:, 32 + j:33 + j]),
                 reads=[("stat2", j)], writes=[("stat2", j)])

        def stB(j):
            ci = 0 if j < NTL else 1
            s2 = j % 2
            xj = xl[:, j, :]
            S.op("dve", lambda o, xj=xj, j=j, ci=ci, s2=s2: o.scalar_tensor_tensor(
                out=t1[s2], in0=xj, scalar=stat[:, 32 + j:33 + j], in1=A[ci], op0=ALU.mult, op1=ALU.mult),
                reads=[("xl", j), ("stat2", j), ("t", id(A[ci]))], writes=[("t1", s2)])
            S.op("dve", lambda o, ci=ci, s2=s2: o.tensor_tensor(out=hb[s2], in0=t1[s2], in1=Sh[ci], op=ALU.add),
                 reads=[("t1", s2), ("t", id(Sh[ci]))], writes=[("hb", s2)])
            if router is None:
                bank = s2
                pv = psb16(bank)[:, 0:D].rearrange("p (c t) -> p c t", c=KC)

                def tr(o, s2=s2, pv=pv):
                    r = None
                    for c in range(KC):
                        r = o.transpose(out=pv[:, c, :], in_=hb[s2][:, c * 128:(c + 1) * 128], identity=idb)
                    return r
                S.op("pe", tr, reads=[("hb", s2)], writes=[("ps", bank)])
            else:
                b0 = 2 * s2
                pv = ps[:, b0:b0 + 2, :].rearrange("p a (c t) -> p (a c) t", c=4)

                def tr(o, s2=s2, pv=pv):
                    r = None
                    for c in range(KC):
                        r = o.transpose(out=pv[:, c, :], in_=hb[s2][:, c * 128:(c + 1) * 128], identity=idf)
                    return r
                S.op("pe", tr, reads=[("hb", s2)], writes=[("ps", b0), ("ps", b0 + 1)])

        def stC(j):
            s2 = j % 2
            if router is None:
                bank = s2
                pv = psb16(bank)[:, 0:D].rearrange("p (c t) -> p c t", c=KC)
                S.op("act", lambda o, pv=pv, j=j: o.copy(out=hT[:, :, j * 128:(j + 1) * 128], in_=pv),
                     reads=[("ps", bank)], writes=[("hT", j)])
            else:
                b0 = 2 * s2
                pv = ps[:, b0:b0 + 2, :].rearrange("p a (c t) -> p (a c) t", c=4)
                S.op("act", lambda o, pv=pv, j=j: o.copy(out=hT[:, :, j * 128:(j + 1) * 128], in_=pv),
                     reads=[("ps", b0), ("ps", b0 + 1)], writes=[("hT", j)])
                S.op("dve", lambda o, pv=pv, s2=s2: o.tensor_copy(out=hTf[s2], in_=pv),
                     reads=[("ps", b0), ("ps", b0 + 1)], writes=[("hTf", s2)])
                lb = 4 + s2

                def lgm(o, s2=s2, lb=lb):
                    r = None
                    for c in range(KC):
                        r = o.matmul(ps[:, lb, 0:NE], lhsT=hTf[s2][:, c, :], rhs=rw[:, c, :],
                                     start=(c == 0), stop=(c == KC - 1))
                    return r
                S.op("pe", lgm, reads=[("hTf", s2), "rw"], writes=[("ps", lb)])
                S.op("dve", lambda o, lb=lb, j=j: o.tensor_copy(out=lg[:, j, :], in_=ps[:, lb, 0:NE]),
                     reads=[("ps", lb)], writes=[("lg", j)])
                S.op("dve", lambda o, j=j: o.max(out=m8[:, j, :], in_=lg[:, j, :]), reads=[("lg", j)], writes=[("m8", j)])

        def router_epilogue(ntl):
            rk = [("lg", j) for j in range(ntl)] + [("m8", j) for j in range(ntl)]
            S.op("dve", lambda o: o.tensor_tensor(out=wgt[:, 0:ntl, 0:1], in0=m8[:, 0:ntl, 0:1], in1=m8[:, 0:ntl, 1:2], op=ALU.subtract),
                 reads=rk, writes=["wg0"])
            S.op("act", lambda o: o.activation(out=wgt[:, 0:ntl, 1:2], in_=wgt[:, 0:ntl, 0:1], func=AF.Sigmoid), reads=["wg0"], writes=["wg1"])
            S.op("dve", lambda o: o.tensor_scalar(out=wgt[:, 0:ntl, 2:3], in0=wgt[:, 0:ntl, 1:2], scalar1=-1.0, scalar2=1.0,
                                                  op0=ALU.mult, op1=ALU.add), reads=["wg1"], writes=["wg2"])
            S.op("dve", lambda o: o.tensor_tensor(out=e1[:, 0:ntl, :], in0=lg[:, 0:ntl, :], in1=m8[:, 0:ntl, 0:1].broadcast_to([128, ntl, NE]),
                                                  op=ALU.is_equal), reads=rk, writes=["e1"])
            S.op("dve", lambda o: o.tensor_tensor(out=e1[:, 0:ntl, :], in0=e1[:, 0:ntl, :], in1=wgt[:, 0:ntl, 1:2].broadcast_to([128, ntl, NE]),
                                                  op=ALU.mult), reads=["e1", "wg1"], writes=["e1"])
            S.op("dve", lambda o: o.tensor_tensor(out=comb[:, 0:ntl, :], in0=lg[:, 0:ntl, :], in1=m8[:, 0:ntl, 1:2].broadcast_to([128, ntl, NE]),
                                                  op=ALU.is_equal), reads=rk, writes=["combx"])
            S.op("dve", lambda o: o.tensor_tensor(out=comb[:, 0:ntl, :], in0=comb[:, 0:ntl, :], in1=wgt[:, 0:ntl, 2:3].broadcast_to([128, ntl, NE]),
                                                  op=ALU.mult), reads=["combx", "wg2"], writes=["combx"])
            S.op("dve", lambda o: o.tensor_tensor(out=comb[:, 0:ntl, :], in0=comb[:, 0:ntl, :], in1=e1[:, 0:ntl, :], op=ALU.add),
                 reads=["combx", "e1"], writes=[("comb", j) for j in range(NT)])

        tl = tiles_for(need_ctx)
        for step in range(len(tl) + 2):
            if step < len(tl):
                stA(tl[step])
            if 0 <= step - 1 < len(tl):
                stB(tl[step - 1])
            if 0 <= step - 2 < len(tl):
                stC(tl[step - 2])
        if router is not None:
            router_epilogue(len(tl))
        S.barrier()
        ar.release(m)

    def resid_update(j, c0, cw, psv, gate, tmp, key, comb_ap=None, extra_reads=()):
        if comb_ap is None:
            S.op("dve", lambda o: o.tensor_tensor(out=tmp, in0=psv, in1=gate[:, c0:c0 + cw], op=ALU.mult),
                 reads=list(extra_reads) + [("t", id(gate))], writes=[key])
        else:
            S.op("dve", lambda o: o.scalar_tensor_tensor(out=tmp, in0=psv, scalar=comb_ap, in1=gate[:, c0:c0 + cw],
                                                         op0=ALU.mult, op1=ALU.mult),
                 reads=list(extra_reads) + [("t", id(gate)), ("comb", j)], writes=[key])
        S.op("pool", lambda o: o.tensor_tensor(out=xl[:, j, c0:c0 + cw], in0=xl[:, j, c0:c0 + cw], in1=tmp, op=ALU.add),
             reads=[key, ("xl", j)], writes=[("xl", j)])

    def wout_phase(l, b, w_src, need_ctx):
        m = ar.mark()
        wo = ar.alloc([128, KC, D], BF16)
        gate = [ar.alloc([128, D], F32) for _ in range(2)]
        tmp = [ar.alloc([128, D], F32) for _ in range(2)]
        wv = w_src.rearrange("(k p) n -> p k n", p=128)
        for h in range(2):
            S.dma("pool", wo[:, :, h * 512:(h + 1) * 512], wv[:, :, h * 512:(h + 1) * 512], writes=[("wo", h)])
        load_mod(gate[0], l, b, 2)
        if need_ctx:
            load_mod(gate[1], l, 2, 2)
        for j in tiles_for(need_ctx):
            ci = 0 if j < NTL else 1
            s2 = j % 2
            b0 = 2 * s2

            def mm(o, j=j, b0=b0):
                r = None
                for h in range(2):
                    for c in range(KC):
                        r = o.matmul(ps[:, b0 + h, :], lhsT=hT[:, c, j * 128:(j + 1) * 128],
                                     rhs=wo[:, c, h * 512:(h + 1) * 512], start=(c == 0), stop=(c == KC - 1))
                return r
            S.op("pe", mm, reads=[("hT", j), ("wo", 0), ("wo", 1)], writes=[("ps", b0), ("ps", b0 + 1)])
            psv = ps[:, b0:b0 + 2, :].rearrange("p a n -> p (a n)")
            resid_update(j, 0, D, psv, gate[ci], tmp[s2], ("tmp", s2), extra_reads=[("ps", b0), ("ps", b0 + 1)])
        S.barrier()
        ar.release(m)

    def ffn_phase(l, b, need_ctx, moe):
        m = ar.mark()
        i = l // 2
        NF = 4
        NS = 2
        wg = [ar.alloc([128, KC, NF * 128], BF16) for _ in range(NS)]
        wu = [ar.alloc([128, KC, NF * 128], BF16) for _ in range(NS)]
        wd = [ar.alloc([128, NF, D], BF16) for _ in range(NS)]
        gate = [ar.alloc([128, D], F32) for _ in range(2)]
        sg = [ar.alloc([128, 512], F32) for _ in range(2)]
        actT = [[ar.alloc([128, 512], BF16) for _ in range(NF)] for _ in range(2)]
        tmp = [ar.alloc([128, 512], F32) for _ in range(4)]
        load_mod(gate[0], l, b, 5)
        if need_ctx:
            load_mod(gate[1], l, 2, 5)
        blocks = blocks_for(need_ctx)
        ngroups = NFC // NF
        experts = list(range(NE)) if moe else [None]
        items = [(e, g) for e in experts for g in range(ngroups)]

        def load(idx):
            e, g = items[idx]
            s = idx % NS
            if moe:
                g_src, u_src, d_src = odwg_d[i, e], odwu_d[i, e], odwd_d[i, e]
            else:
                g_src, u_src, d_src = evwg_d[i], evwu_d[i], evwd_d[i]
            f0 = g * NF * 128
            S.dma("pool", wg[s], g_src.rearrange("(k p) f -> p k f", p=128)[:, :, f0:f0 + NF * 128], writes=[("wg", s)])
            S.dma("pool", wu[s], u_src.rearrange("(k p) f -> p k f", p=128)[:, :, f0:f0 + NF * 128], writes=[("wu", s)])
            S.dma("pool", wd[s], d_src[f0:f0 + NF * 128, :].rearrange("(f p) d -> p f d", p=128), writes=[("wd", s)])

        for idx in range(min(NS - 1, len(items))):
            load(idx)
        cnt = [0]
        pend = [None]

        def phaseB(args):
            e, s, t0, tw, aset = args
            for qt in range(tw // 128):
                j = (t0 // 128) + qt
                ci = 0 if j < NTL else 1
                for h in range(2):
                    k = cnt[0] % 4
                    cnt[0] += 1
                    bank = 4 + k

                    def mm(o, qt=qt, h=h, bank=bank, s=s, aset=aset):
                        r = None
                        for fi in range(NF):
                            r = o.matmul(ps[:, bank, :], lhsT=actT[aset][fi][:, qt * 128:(qt + 1) * 128],
                                         rhs=wd[s][:, fi, h * 512:(h + 1) * 512], start=(fi == 0), stop=(fi == NF - 1))
                        return r
                    S.op("pe", mm, reads=[("actT", aset, fi) for fi in range(NF)] + [("wd", s)], writes=[("ps", bank)])
                    cap = comb[:, j, e:e + 1] if moe else None
                    resid_update(j, h * 512, 512, ps[:, bank, :], gate[ci], tmp[k], ("tmp", k), comb_ap=cap,
                                 extra_reads=[("ps", bank)])

        ab = 0
        for idx, (e, g) in enumerate(items):
            if pend[0] is not None:
                phaseB(pend[0])
                pend[0] = None
            if idx + NS - 1 < len(items):
                load(idx + NS - 1)
            s = idx % NS
            for (t0, tw) in blocks:
                aset = ab % 2
                ab += 1
                for fi in range(NF):
                    pb = 2 * (fi % 2)

                    def mmA(o, fi=fi, pb=pb, s=s, t0=t0, tw=tw):
                        r = None
                        for (wsrc, bk) in ((wg[s], pb), (wu[s], pb + 1)):
                            for c in range(KC):
                                r = o.matmul(ps[:, bk, 0:tw], lhsT=wsrc[:, c, fi * 128:(fi + 1) * 128],
                                             rhs=hT[:, c, t0:t0 + tw], start=(c == 0), stop=(c == KC - 1))
                        return r
                    S.op("pe", mmA, reads=[("wg", s), ("wu", s)] + [("hT", jj) for jj in range(t0 // 128, (t0 + tw) // 128)],
                         writes=[("ps", pb), ("ps", pb + 1)])
                    s2 = fi % 2
                    S.op("act", lambda o, pb=pb, s2=s2, tw=tw: o.activation(out=sg[s2][:, 0:tw], in_=ps[:, pb, 0:tw], func=AF.Silu),
                         reads=[("ps", pb)], writes=[("sg", s2)])
                    S.op("dve", lambda o, pb=pb, s2=s2, tw=tw, aset=aset, fi=fi: o.tensor_tensor(
                        out=actT[aset][fi][:, 0:tw], in0=sg[s2][:, 0:tw], in1=ps[:, pb + 1, 0:tw], op=ALU.mult),
                        reads=[("sg", s2), ("ps", pb + 1)], writes=[("actT", aset, fi)])
                if pend[0] is not None:
                    phaseB(pend[0])
                pend[0] = (e, s, t0, tw, aset)
        if pend[0] is not None:
            phaseB(pend[0])
        S.barrier()
        ar.release(m)

    def even_mixer(l, b, need_ctx):
        i = l // 2
        m = ar.mark()
        QK = ar.alloc([128, 4, T], BF16)
        KM = ar.alloc([128, 2, T], BF16)
        V = ar.alloc([128, NT, 2, HD + 1], BF16)
        yT = ar.alloc([128, 4, YT_W], BF16)
        cwT = ar.alloc([128, 4, 33], F32)
        m_attn = ar.mark()
        w_in = ar.alloc([128, KC, 768], BF16)
        gq = ar.alloc([128, 10, HD], F32)
        gsrc = ar.alloc([128, 2, HD], F32)
        sq = [ar.alloc([128, 640], F32) for _ in range(2)]
        qk = [ar.alloc([128, 640], F32) for _ in range(2)]
        rt = [ar.alloc([128, 320], F32) for _ in range(4)]
        qkb = [ar.alloc([128, 640], BF16) for _ in range(2)]
        st2 = ar.alloc([128, NT, 10], F32)
        cw33 = ar.alloc([128, 512], F32)
        wv = evwin_d[i].rearrange("(k p) n -> p k n", p=128)
        for kv in range(2):
            for ii in range(4):
                S.dma("pool", w_in[:, :, (ii * 2 + kv) * 64:(ii * 2 + kv + 1) * 64],
                      wv[:, :, 1024 + (kv * 4 + ii) * 64:1024 + (kv * 4 + ii + 1) * 64], writes=[("win", 2 + kv, ii)])
        S.dma("pool", w_in[:, :, 512:768], wv[:, :, 1536:1792], writes=[("win", 4)])
        S.dma("sp", gsrc[:, 0, :], evqg_d[i].partition_broadcast(128), writes=["gsrc0"])
        S.dma("sp", gsrc[:, 1, :], evkg_d[i].partition_broadcast(128), writes=["gsrc1"])
        S.op("dve", lambda o: o.tensor_copy(out=gq[:, 0:8, :], in_=gsrc[:, 0:1, :].broadcast_to([128, 8, HD])), reads=["gsrc0"], writes=["gq"])
        S.op("dve", lambda o: o.tensor_copy(out=gq[:, 8:10, :], in_=gsrc[:, 1:2, :].broadcast_to([128, 2, HD])), reads=["gsrc1"], writes=["gq2"])
        S.dma("sp", cw33[0:31, :], evcw_d[i], writes=["cw33a"])
        S.dma("sp", cw33[31:32, :], evlng_d[i].unsqueeze(0), writes=["cw33b"])
        S.dma("sp", cw33[32:33, :], evlnb_d[i].unsqueeze(0), writes=["cw33c"])
        S.op("dve", lambda o: o.memset(V, 1.0), writes=["V"])
        S.op("dve", lambda o: o.memset(KM, 0.0), writes=["KM"])
        S.op("dve", lambda o: o.memset(yT, 0.0), writes=["yT"])

        def trc(o):
            r = None
            for cc in range(4):
                r = o.transpose(out=ps[:, 7, cc * 64:cc * 64 + 33], in_=cw33[0:33, cc * 128:(cc + 1) * 128],
                                identity=idf[0:33, 0:33])
            return r
        S.op("pe", trc, reads=["cw33a", "cw33b", "cw33c"], writes=[("ps", 7)])
        S.op("dve", lambda o: o.tensor_copy(out=cwT, in_=ps[:, 7, 0:256].rearrange("p (c w) -> p c w", c=4)[:, :, 0:33]),
             reads=[("ps", 7)], writes=["cwT"])
        S.barrier()
        def p1(j):
            s2 = j % 2
            b0 = 2 * s2
            lat = j < NTL

            def mm(o, j=j, b0=b0):
                r = None
                for (bk, c0, cw_) in ((b0, 0, 512), (b0 + 1, 512, 256)):
                    for c in range(KC):
                        r = o.matmul(ps[:, bk, 0:cw_], lhsT=hT[:, c, j * 128:(j + 1) * 128], rhs=w_in[:, c, c0:c0 + cw_],
                                     start=(c == 0), stop=(c == KC - 1))
                return r
            S.op("pe", mm, reads=[("hT", j), ("win", 4)] + [("win", 2 + kv_, i_) for kv_ in range(2) for i_ in range(4)], writes=[("ps", b0), ("ps", b0 + 1)])
            pq = ps[:, b0:b0 + 2, :].rearrange("p a n -> p (a n)")[:, 0:640]
            S.op("act", lambda o, pq=pq, s2=s2: o.activation(out=sq[s2], in_=pq, func=AF.Square),
                 reads=[("ps", b0), ("ps", b0 + 1)], writes=[("sq", s2)])
            S.op("dve", lambda o, s2=s2, j=j: o.tensor_reduce(out=st2[:, j, :], in_=sq[s2].rearrange("p (h d) -> p h d", h=10),
                                                              axis=AX.X, op=ALU.add), reads=[("sq", s2)], writes=[("st2", j)])
            S.op("act", lambda o, j=j: o.activation(out=st2[:, j, :], in_=st2[:, j, :], func=AF.Sqrt, bias=1e-6, scale=1.0 / HD),
                 reads=[("st2", j)], writes=[("st2", j)])
            S.op("dve", lambda o, j=j: o.reciprocal(out=st2[:, j, :], in_=st2[:, j, :]), reads=[("st2", j)], writes=[("st2", j)])
            qk3 = qk[s2].rearrange("p (h d) -> p h d", h=10)
            S.op("dve", lambda o, pq=pq, qk3=qk3, j=j: o.tensor_tensor(
                out=qk3, in0=pq.rearrange("p (h d) -> p h d", h=10), in1=st2[:, j, :].unsqueeze(2).broadcast_to([128, 10, HD]),
                op=ALU.mult), reads=[("ps", b0), ("ps", b0 + 1), ("st2", j)], writes=[("qk", s2)])
            S.op("dve", lambda o, qk3=qk3: o.tensor_tensor(out=qk3, in0=qk3, in1=gq, op=ALU.mult),
                 reads=[("qk", s2), "gq", "gq2"], writes=[("qk", s2)])
            if lat:
                rope(qk[s2], qkb[s2], 10, j, rt, ("qk", s2), ("qkb", s2))
            else:
                S.op("dve", lambda o, s2=s2: o.tensor_copy(out=qkb[s2], in_=qk[s2]), reads=[("qk", s2)], writes=[("qkb", s2)])
            S.op("act", lambda o, b0=b0, j=j: o.copy(out=V[:, j, :, 0:HD], in_=ps[:, b0 + 1, 128:256].rearrange("p (a d) -> p a d", a=2)),
                 reads=[("ps", b0 + 1)], writes=[("V", j)])
        def p2(j):
            s2 = j % 2
            lat = j < NTL
            do_q = lat or need_ctx
            tb = 4 + s2
            pv = psb16(tb)[:, 0:640].rearrange("p (c t) -> p c t", c=5)

            def tr(o, s2=s2, pv=pv, do_q=do_q):
                r = None
                for c in (range(5) if do_q else [4]):
                    r = o.transpose(out=pv[:, c, :], in_=qkb[s2][:, c * 128:(c + 1) * 128], identity=idb)
                return r
            S.op("pe", tr, reads=[("qkb", s2)], writes=[("ps", tb)])
            if do_q:
                S.op("act", lambda o, pv=pv, j=j: o.copy(out=QK[:, :, j * 128:(j + 1) * 128], in_=pv[:, 0:4, :]),
                     reads=[("ps", tb)], writes=[("QK", j)])
            for kv_ in range(2):
                S.op("act", lambda o, pv=pv, j=j, kv_=kv_: o.copy(out=KM[kv_ * 64:(kv_ + 1) * 64, kv_, j * 128:(j + 1) * 128],
                                                                in_=pv[kv_ * 64:(kv_ + 1) * 64, 4, :]),
                     reads=[("ps", tb)], writes=[("KM", kv_, j)])
        for k in range(NT + 1):
            if k < NT:
                p1(k)
            if k >= 1:
                p2(k - 1)
        S.barrier()
        ar.release(m_attn)
        w_ag = ar.alloc([128, KC, 1024], BF16)
        sgm = [ar.alloc([128, 512], F32) for _ in range(2)]
        S.dma("pool", w_ag[:, :, 0:512], wv[:, :, 0:512], writes=[("win", 0)])
        S.dma("pool", w_ag[:, :, 512:1024], wv[:, :, 512:1024], writes=[("win", 1)])
        k_ = 0
        for (t0, tw) in blocks_for(need_ctx):
            yoff = (PAD + t0) if t0 < TL else (CTX_OFF + t0 - TL)
            for cc in range(4):
                pb = 2 * (k_ % 2)
                s2 = k_ % 2
                k_ += 1

                def mm(o, cc=cc, pb=pb, t0=t0, tw=tw):
                    r = None
                    for (bk, c0) in ((pb, cc * 128), (pb + 1, 512 + cc * 128)):
                        for c in range(KC):
                            r = o.matmul(ps[:, bk, 0:tw], lhsT=w_ag[:, c, c0:c0 + 128], rhs=hT[:, c, t0:t0 + tw],
                                         start=(c == 0), stop=(c == KC - 1))
                    return r
                S.op("pe", mm, reads=[("win", 0), ("win", 1)] + [("hT", jj) for jj in range(t0 // 128, (t0 + tw) // 128)],
                     writes=[("ps", pb), ("ps", pb + 1)])
                S.op("act", lambda o, pb=pb, s2=s2, tw=tw: o.activation(out=sgm[s2][:, 0:tw], in_=ps[:, pb + 1, 0:tw], func=AF.Sigmoid),
                     reads=[("ps", pb + 1)], writes=[("sgm", s2)])
                S.op("dve", lambda o, pb=pb, s2=s2, tw=tw, cc=cc, yoff=yoff: o.tensor_tensor(
                    out=yT[:, cc, yoff:yoff + tw], in0=sgm[s2][:, 0:tw], in1=ps[:, pb, 0:tw], op=ALU.mult),
                    reads=[("sgm", s2), ("ps", pb)], writes=["yT"])
        S.barrier()
        ar.release(m_attn)
        PT = [ar.alloc([128, 512], BF16) for _ in range(4)]
        att = [ar.alloc([128, 4, 512], BF16) for _ in range(2)]
        rl = [ar.alloc([128, 4], F32) for _ in range(2)]
        convF = ar.alloc([128, 4, 512], F32)
        sqF = ar.alloc([128, 4, 512], F32)
        mean = ar.alloc([128, 512], F32)
        var = ar.alloc([128, 512], F32)
        z = [ar.alloc([128, 512], F32) for _ in range(2)]

        def conv_gen():
            for (t0, tw) in blocks_for(need_ctx):
                yoff = (PAD + t0) if t0 < TL else (CTX_OFF + t0 - TL)
                for tp in range(CONVW):
                    for cc in range(4):
                        eng = "dve"
                        if tp == 0:
                            S.op(eng, lambda o, cc=cc, yoff=yoff, tw=tw: o.tensor_scalar(
                                out=convF[:, cc, 0:tw], in0=yT[:, cc, yoff - PAD:yoff - PAD + tw], scalar1=cwT[:, cc, 0:1], scalar2=None,
                                op0=ALU.mult), reads=["yT", "cwT"], writes=[("convF", cc)])
                        elif cc < 4:
                            S.op(eng, lambda o, cc=cc, yoff=yoff, tw=tw, tp=tp: o.scalar_tensor_tensor(
                                out=convF[:, cc, 0:tw], in0=yT[:, cc, yoff - PAD + tp:yoff - PAD + tp + tw], scalar=cwT[:, cc, tp:tp + 1],
                                in1=convF[:, cc, 0:tw], op0=ALU.mult, op1=ALU.add), reads=[("convF", cc)], writes=[("convF", cc)])
                        else:
                            k2 = tp % 2
                            S.op(eng, lambda o, cc=cc, yoff=yoff, tw=tw, tp=tp, k2=k2: o.tensor_scalar(
                                out=ptmp[k2][:, 0:tw], in0=yT[:, cc, yoff - PAD + tp:yoff - PAD + tp + tw], scalar1=cwT[:, cc, tp:tp + 1],
                                scalar2=None, op0=ALU.mult), reads=["yT", "cwT"], writes=[("ptmp", k2)])
                            S.op(eng, lambda o, cc=cc, tw=tw, k2=k2: o.tensor_tensor(
                                out=convF[:, cc, 0:tw], in0=convF[:, cc, 0:tw], in1=ptmp[k2][:, 0:tw], op=ALU.add),
                                reads=[("convF", cc), ("ptmp", k2)], writes=[("convF", cc)])
                    yield
                S.op("act", lambda o, tw=tw: o.activation(out=sqF[:, :, 0:tw], in_=convF[:, :, 0:tw], func=AF.Square),
                     reads=[("convF", cc) for cc in range(4)], writes=["sqF"])

                def mst(o, tw=tw):
                    r = None
                    for (bk, src) in ((4, convF), (5, sqF)):
                        for cc in range(4):
                            r = o.matmul(ps[:, bk, 0:tw], lhsT=onesN, rhs=src[:, cc, 0:tw], start=(cc == 0), stop=(cc == 3))
                    return r
                S.op("pe", mst, reads=[("convF", cc) for cc in range(4)] + ["sqF", "onesN"], writes=[("ps", 4), ("ps", 5)])
                S.op("act", lambda o, tw=tw: o.copy(out=mean[:, 0:tw], in_=ps[:, 4, 0:tw]), reads=[("ps", 4)], writes=["mean"])
                S.op("dve", lambda o, tw=tw: o.tensor_tensor(out=var[:, 0:tw], in0=mean[:, 0:tw], in1=mean[:, 0:tw], op=ALU.mult),
                     reads=["mean"], writes=["var"])
                S.op("dve", lambda o, tw=tw: o.tensor_tensor(out=var[:, 0:tw], in0=ps[:, 5, 0:tw], in1=var[:, 0:tw], op=ALU.subtract),
                     reads=["var", ("ps", 5)], writes=["var"])
                S.op("act", lambda o, tw=tw: o.activation(out=var[:, 0:tw], in_=var[:, 0:tw], func=AF.Sqrt, bias=1e-5, scale=1.0),
                     reads=["var"], writes=["var"])
                S.op("dve", lambda o, tw=tw: o.reciprocal(out=var[:, 0:tw], in_=var[:, 0:tw]), reads=["var"], writes=["var"])
                yield
                for cc in range(4):
                    s2 = cc % 2
                    S.op("dve", lambda o, cc=cc, s2=s2, tw=tw: o.tensor_tensor(out=z[s2][:, 0:tw], in0=convF[:, cc, 0:tw], in1=mean[:, 0:tw],
                                                                             op=ALU.subtract), reads=[("convF", cc), "mean"], writes=[("z", s2)])
                    S.op("dve", lambda o, s2=s2, tw=tw: o.tensor_tensor(out=z[s2][:, 0:tw], in0=z[s2][:, 0:tw], in1=var[:, 0:tw], op=ALU.mult),
                         reads=[("z", s2), "var"], writes=[("z", s2)])
                    S.op("act", lambda o, cc=cc, s2=s2, tw=tw, t0=t0: o.activation(
                        out=hT[:, cc, t0:t0 + tw], in_=z[s2][:, 0:tw], func=AF.Silu, bias=cwT[:, cc, 32:33], scale=cwT[:, cc, 31:32]),
                        reads=[("z", s2), "cwT"], writes=[("hTc", cc, t0)])
                    yield

        ga = attention_gqa(QK, KM, V, PT, att, rl, need_ctx)
        gc = conv_gen()
        a_alive = c_alive = True
        while a_alive or c_alive:
            for _r in range(4):
                if a_alive:
                    try:
                        next(ga)
                    except StopIteration:
                        a_alive = False
            if c_alive:
                try:
                    next(gc)
                except StopIteration:
                    c_alive = False
        S.barrier()
        ar.release(m)
        wout_phase(l, b, evwout_d[i], need_ctx)

    def rope(src, dst, nh, j, rt, rkey, wkey):
        sv = src.rearrange("p (h a t f) -> p h a t f", h=nh, a=2, t=2)
        dv = dst.rearrange("p (h a t f) -> p h a t f", h=nh, a=2, t=2)
        x1, x2 = sv[:, :, :, 0, :], sv[:, :, :, 1, :]
        cb = cos_t[:, j, :].rearrange("p (a f) -> p a f", a=2).unsqueeze(1).broadcast_to([128, nh, 2, 16])
        sb = sin_t[:, j, :].rearrange("p (a f) -> p a f", a=2).unsqueeze(1).broadcast_to([128, nh, 2, 16])
        n = nh * 32
        r = [t[:, 0:n].rearrange("p (h a f) -> p h a f", h=nh, a=2) for t in rt]
        S.op("dve", lambda o: o.tensor_tensor(out=r[0], in0=x1, in1=cb, op=ALU.mult), reads=[rkey, "cos"], writes=[("rt", 0)])
        S.op("pool", lambda o: o.tensor_tensor(out=r[1], in0=x2, in1=sb, op=ALU.mult), reads=[rkey, "sin"], writes=[("rt", 1)])
        S.op("dve", lambda o: o.tensor_tensor(out=r[2], in0=x2, in1=cb, op=ALU.mult), reads=[rkey, "cos"], writes=[("rt", 2)])
        S.op("pool", lambda o: o.tensor_tensor(out=r[3], in0=x1, in1=sb, op=ALU.mult), reads=[rkey, "sin"], writes=[("rt", 3)])
        S.op("dve", lambda o: o.tensor_tensor(out=dv[:, :, :, 0, :], in0=r[0], in1=r[1], op=ALU.subtract),
             reads=[("rt", 0), ("rt", 1)], writes=[(wkey, 0)])
        S.op("dve", lambda o: o.tensor_tensor(out=dv[:, :, :, 1, :], in0=r[2], in1=r[3], op=ALU.add),
             reads=[("rt", 2), ("rt", 3)], writes=[wkey])

    def attention_gqa(QK, KM, V, PT, att, rl, need_ctx):
        qblocks = [(i * 512, 512, list(range(NT))) for i in range(4)]
        if need_ctx:
            qblocks.append((TL, TC, [16, 17]))
        step = [0]
        sctr = [0]
        for bi, (q0, qw, keys) in enumerate(qblocks):
            nqt = qw // 128
            a2 = bi % 2
            for h in range(8):
                kv, ii = h // 4, h % 4
                ob = 2 + (step[0] % 2)
                step[0] += 1
                p0 = kv * 64
                ov = ps[:, ob, :].rearrange("p (q n) -> p q n", q=4)

                def sT(o, s, sb_, kv=kv, ii=ii, q0=q0, qw=qw):
                    return o.matmul(ps[:, sb_, 0:qw], lhsT=KM[:, kv, s * 128:(s + 1) * 128],
                                    rhs=QK[:, ii, q0:q0 + qw], start=True, stop=True)

                def pvm(o, s, pt, first, last, kv=kv, ov=ov, nqt=nqt):
                    r = None
                    for qt in range(nqt):
                        r = o.matmul(ov[:, qt, 0:HD + 1], lhsT=pt[:, qt * 128:(qt + 1) * 128], rhs=V[:, s, kv, :],
                                     start=(first and qt == 0), stop=last, skip_group_check=True)
                    return r
                nk = len(keys)
                LA = 2
                SB = (0, 1, 6, 7)
                pts = []
                for si in range(nk + LA):
                    yield
                    if si < nk:
                        s = keys[si]
                        sb_ = SB[sctr[0] % 4]
                        pi = sctr[0] % 4
                        sctr[0] += 1
                        pts.append(pi)
                        S.op("pe", lambda o, s=s, sb_=sb_, sT=sT: sT(o, s, sb_),
                             reads=[("KM", 0, s), ("KM", 1, s)] + [("QK", jj) for jj in range(q0 // 128, (q0 + qw) // 128)], writes=[("ps", sb_)])
                        pt = PT[pi]
                        S.op("act", lambda o, sb_=sb_, pt=pt, qw=qw: o.activation(out=pt[:, 0:qw], in_=ps[:, sb_, 0:qw], func=AF.Exp,
                                                                               scale=HD ** -0.5),
                             reads=[("ps", sb_)], writes=[("PT", pi)])
                    if si >= LA:
                        sp_ = keys[si - LA]
                        pi2 = pts[si - LA]
                        pt = PT[pi2]
                        S.op("pe", lambda o, sp_=sp_, pt=pt, first=(si == LA), last=(si == nk + LA - 1), pvm=pvm: pvm(o, sp_, pt, first, last),
                             reads=[("PT", pi2), ("V", sp_)], writes=[("ps", ob)])
                S.op("dve", lambda o, ov=ov, a2=a2, nqt=nqt: o.reciprocal(out=rl[a2][:, 0:nqt], in_=ov[:, 0:nqt, HD]),
                     reads=[("ps", ob)], writes=[("rl", a2)])
                S.op("dve", lambda o, ov=ov, a2=a2, nqt=nqt, h=h: o.tensor_tensor(
                    out=att[a2][:, 0:nqt, h * HD:(h + 1) * HD], in0=ov[:, 0:nqt, 0:HD],
                    in1=rl[a2][:, 0:nqt].unsqueeze(2).broadcast_to([128, nqt, HD]), op=ALU.mult),
                    reads=[("ps", ob), ("rl", a2)], writes=[("att", a2)])
            tb0 = 4
            pvv = ps[:, tb0:tb0 + 2, :].bitcast(BF16).rearrange("p a (c t) -> p (a c) t", c=2)[:, :, 0:qw]

            def tr(o, a2=a2, pvv=pvv, nqt=nqt):
                r = None
                for c in range(4):
                    for qt in range(nqt):
                        r = o.transpose(out=pvv[:, c, qt * 128:(qt + 1) * 128], in_=att[a2][:, qt, c * 128:(c + 1) * 128], identity=idb)
                return r
            S.op("pe", tr, reads=[("att", a2)], writes=[("ps", tb0), ("ps", tb0 + 1)])
            S.op("act", lambda o, pvv=pvv, q0=q0, qw=qw: o.copy(out=hT[:, 4:8, q0:q0 + qw], in_=pvv),
                 reads=[("ps", tb0), ("ps", tb0 + 1)], writes=[("hTa", q0)])

    def odd_mixer(l, b, need_ctx):
        i = l // 2
        lam_init = 0.8 - 0.6 * float(np.exp(-0.3 * l))
        m = ar.mark()
        mixT = ar.alloc([128, KC, T], BF16)
        m2 = ar.mark()
        wh = [ar.alloc([128, KC, 384], BF16) for _ in range(2)]
        QKh = [ar.alloc([128, 2, T], BF16) for _ in range(2)]
        Vh = [ar.alloc([128, NT, 129], BF16) for _ in range(2)]
        PT = [[ar.alloc([128, 512], BF16) for _ in range(2)] for _ in range(2)]
        qf = [ar.alloc([128, 256], F32) for _ in range(2)]
        qkb = [ar.alloc([128, 256], BF16) for _ in range(2)]
        rt = [ar.alloc([128, 128], F32) for _ in range(4)]
        lamt = ar.alloc([128, 4, HD], F32)
        lt = ar.alloc([128, 8], F32)
        sgb = ar.alloc([128, 128], F32)
        rlh = [ar.alloc([128, 4], F32) for _ in range(2)]
        t0f = [ar.alloc([128, 2, 128], F32) for _ in range(2)]
        sqh = [ar.alloc([128, 2, 128], F32) for _ in range(2)]
        ob_ = [ar.alloc([128, 2, 128], BF16) for _ in range(2)]
        wv = odwin_d[i].rearrange("(k p) n -> p k n", p=128)

        def loadw(h):
            s = h % 2
            for part in range(3):
                S.dma("pool", wh[s][:, :, part * 128:(part + 1) * 128], wv[:, :, part * 1024 + h * 128:part * 1024 + (h + 1) * 128],
                      writes=[("wh", s, part)])
        loadw(0)
        S.dma("sp", lamt, odlam_d[i].partition_broadcast(128), writes=["lamt"])
        S.dma("sp", sgb, odsg_d[i].partition_broadcast(128), writes=["sgb"])
        S.op("dve", lambda o: o.memset(lt, 0.0), writes=["lt"])
        S.op("dve", lambda o: o.tensor_tensor(out=lamt[:, 0, :], in0=lamt[:, 0, :], in1=lamt[:, 1, :], op=ALU.mult), reads=["lamt"], writes=["lamt"])
        S.op("dve", lambda o: o.tensor_tensor(out=lamt[:, 2, :], in0=lamt[:, 2, :], in1=lamt[:, 3, :], op=ALU.mult), reads=["lamt"], writes=["lamt"])
        S.op("dve", lambda o: o.tensor_reduce(out=lt[:, 0:1], in_=lamt[:, 0, :], axis=AX.X, op=ALU.add), reads=["lamt"], writes=["lt"])
        S.op("dve", lambda o: o.tensor_reduce(out=lt[:, 1:2], in_=lamt[:, 2, :], axis=AX.X, op=ALU.add), reads=["lamt"], writes=["lt"])
        S.op("act", lambda o: o.activation(out=lt[:, 2:4], in_=lt[:, 0:2], func=AF.Exp), reads=["lt"], writes=["lt"])
        S.op("dve", lambda o: o.tensor_tensor(out=lt[:, 4:5], in0=lt[:, 3:4], in1=lt[:, 2:3], op=ALU.subtract), reads=["lt"], writes=["lt"])
        S.op("dve", lambda o: o.tensor_scalar(out=lt[:, 5:6], in0=lt[:, 4:5], scalar1=-lam_init, scalar2=None, op0=ALU.add), reads=["lt"], writes=["lt"])
        S.op("dve", lambda o: o.tensor_scalar(out=sgb, in0=sgb, scalar1=(1.0 - lam_init), scalar2=None, op0=ALU.mult), reads=["sgb"], writes=["sgb"])
        S.op("dve", lambda o: o.memset(Vh[0], 1.0), writes=["Vh0"])
        S.op("dve", lambda o: o.memset(Vh[1], 1.0), writes=["Vh1"])
        S.barrier()
        neglam = lt[:, 5:6]
        qblocks = [(ii * 512, 512, list(range(NT))) for ii in range(4)]
        if need_ctx:
            qblocks.append((TL, TC, [16, 17]))
        def proj_gen(h):
            s = h % 2

            def pmm(j):
                lat = j < NTL
                s2 = j % 2

                def mm(o, j=j, s=s):
                    r = None
                    for c in range(KC):
                        r = o.matmul(ps[:, 6, 0:384], lhsT=hT[:, c, j * 128:(j + 1) * 128], rhs=wh[s][:, c, :],
                                     start=(c == 0), stop=(c == KC - 1))
                    return r
                S.op("pe", mm, reads=[("hT", j)] + [("wh", s, p_) for p_ in range(3)], writes=[("ps", 6)])
                S.op("act", lambda o, j=j: o.copy(out=Vh[s][:, j, 0:128], in_=ps[:, 6, 256:384]), reads=[("ps", 6)], writes=[("Vh", s, j)])
                if lat:
                    S.op("act", lambda o, s2=s2: o.copy(out=qf[s2], in_=ps[:, 6, 0:256]), reads=[("ps", 6)], writes=[("qf", s2)])
                    rope(qf[s2], qkb[s2], 4, j, rt, ("qf", s2), ("qkb", s2))
                else:
                    S.op("act", lambda o, s2=s2: o.copy(out=qkb[s2], in_=ps[:, 6, 0:256]), reads=[("ps", 6)], writes=[("qkb", s2)])

            def ptr(j):
                lat = j < NTL
                do_q = lat or need_ctx
                s2 = j % 2
                pv = psb16(7)[:, 0:256].rearrange("p (c t) -> p c t", c=2)

                def tr(o, s2=s2, pv=pv, do_q=do_q):
                    r = None
                    for c in (range(2) if do_q else [1]):
                        r = o.transpose(out=pv[:, c, :], in_=qkb[s2][:, c * 128:(c + 1) * 128], identity=idb)
                    return r
                S.op("pe", tr, reads=[("qkb", s2)], writes=[("ps", 7)])
                if do_q:
                    S.op("act", lambda o, pv=pv, j=j: o.copy(out=QKh[s][:, :, j * 128:(j + 1) * 128], in_=pv), reads=[("ps", 7)], writes=[("QKh", s, j)])
                else:
                    S.op("act", lambda o, pv=pv, j=j: o.copy(out=QKh[s][:, 1, j * 128:(j + 1) * 128], in_=pv[:, 1, :]), reads=[("ps", 7)], writes=[("QKh", s, j)])
            for k in range(NT + 1):
                if k < NT:
                    pmm(k)
                if k >= 1:
                    ptr(k - 1)
                yield

        def attn_gen(h):
            s = h % 2
            for (q0, qw, keys) in qblocks:
                nqt = qw // 128
                nk = len(keys)
                def ovw(c, qt):
                    return ps[:, 2 + 2 * c + qt // 2, :][:, (qt % 2) * 256:(qt % 2) * 256 + 129]
                for si in range(nk + 1):
                    if si >= 1 and pending:
                        for f_ in pending.pop(0):
                            f_()
                    if si < nk:
                        sk = keys[si]
                        for c in range(2):
                            sbk = (c, 6 + c)[si % 2]
                            S.op("pe", lambda o, sk=sk, c=c, q0=q0, qw=qw, sbk=sbk: o.matmul(
                                ps[:, sbk, 0:qw], lhsT=QKh[s][c * 64:(c + 1) * 64, 1, sk * 128:(sk + 1) * 128],
                                rhs=QKh[s][c * 64:(c + 1) * 64, 0, q0:q0 + qw], start=True, stop=True),
                                reads=[("QKh", s, sk)] + [("QKh", s, jj) for jj in range(q0 // 128, (q0 + qw) // 128)], writes=[("ps", sbk)])
                            pt = PT[c][si % 2]
                            S.op("act", lambda o, c=c, pt=pt, qw=qw, sbk=sbk: o.activation(out=pt[:, 0:qw], in_=ps[:, sbk, 0:qw], func=AF.Exp, scale=HD ** -0.5),
                                 reads=[("ps", sbk)], writes=[("PT", c, si % 2)])
                    if si >= 1:
                        sp_ = keys[si - 1]
                        for c in range(2):
                            pt = PT[c][(si - 1) % 2]

                            def pvm(o, c=c, pt=pt, sp_=sp_, first=(si == 1), last=(si == nk), nqt=nqt, ovw=ovw):
                                r = None
                                for qt in range(nqt):
                                    r = o.matmul(ovw(c, qt), lhsT=pt[:, qt * 128:(qt + 1) * 128], rhs=Vh[s][:, sp_, :],
                                                 start=(first and qt % 2 == 0), stop=last, skip_group_check=True)
                                return r
                            S.op("pe", pvm, reads=[("PT", c, (si - 1) % 2), ("Vh", s, sp_)], writes=[("ps", 2 + 2 * c), ("ps", 3 + 2 * c)])
                for f_ in [f for st in pending for f in st]:
                    f_()
                del pending[:]
                for hf in range(nqt // 2):
                    o0 = ps[:, 2 + hf, :].rearrange("p (q n) -> p q n", q=2)
                    o1 = ps[:, 4 + hf, :].rearrange("p (q n) -> p q n", q=2)
                    s2 = hf % 2
                    rd = [("ps", 2 + hf), ("ps", 4 + hf)]
                    rA = rlh[s2]
                    S.op("dve", lambda o, o0=o0, rA=rA: o.reciprocal(out=rA[:, 0:2], in_=o0[:, :, 128]), reads=rd, writes=[("rl0", s2)])
                    S.op("dve", lambda o, o1=o1, rA=rA: o.reciprocal(out=rA[:, 2:4], in_=o1[:, :, 128]), reads=rd, writes=[("rl1", s2)])
                    S.op("dve", lambda o, o0=o0, s2=s2, rA=rA: o.tensor_tensor(out=t0f[s2], in0=o0[:, :, 0:128],
                                                                              in1=rA[:, 0:2].unsqueeze(2).broadcast_to([128, 2, 128]), op=ALU.mult),
                         reads=rd + [("rl0", s2)], writes=[("t0f", s2)])
                    S.op("dve", lambda o, o1=o1, s2=s2, rA=rA: o.scalar_tensor_tensor(out=sqh[s2], in0=o1[:, :, 0:128], scalar=neglam,
                                                                                     in1=rA[:, 2:4].unsqueeze(2).broadcast_to([128, 2, 128]),
                                                                                     op0=ALU.mult, op1=ALU.mult), reads=rd + [("rl1", s2), "lt"], writes=[("sqo", s2)])
                yield "mid"
                for f_ in [f for st in pending for f in st]:
                    f_()
                del pending[:]
                stages = [[] for _ in range(7)]
                for hf in range(nqt // 2):
                    s2 = hf % 2
                    rA = rlh[s2]
                    tq = q0 + hf * 256
                    pv = psb16(7)[:, 0:256].rearrange("p (c t) -> p c t", c=2)

                    def tr(o, s2=s2, pv=pv):
                        r = None
                        for qt in range(2):
                            r = o.transpose(out=pv[:, qt, :], in_=ob_[s2][:, qt, :], identity=idb)
                        return r
                    stages[0].append(lambda s2=s2: S.op("dve", lambda o, s2=s2: o.tensor_tensor(out=t0f[s2], in0=t0f[s2], in1=sqh[s2], op=ALU.add),
                                                        reads=[("t0f", s2), ("sqo", s2)], writes=[("t0f", s2)]))
                    stages[1].append(lambda s2=s2: S.op("act", lambda o, s2=s2: o.activation(out=sqh[s2], in_=t0f[s2], func=AF.Square),
                                                        reads=[("t0f", s2)], writes=[("sqo", s2)]))
                    stages[2].append(lambda s2=s2, rA=rA: S.op("dve", lambda o, s2=s2, rA=rA: o.tensor_reduce(out=rA[:, 0:2], in_=sqh[s2], axis=AX.X, op=ALU.add),
                                                               reads=[("sqo", s2), ("rl0", s2)], writes=[("rl0", s2)]))
                    stages[3].append(lambda s2=s2, rA=rA: S.op("act", lambda o, rA=rA: o.activation(out=rA[:, 0:2], in_=rA[:, 0:2], func=AF.Ln, bias=1e-6, scale=1.0 / 128),
                                                               reads=[("rl0", s2)], writes=[("rl0", s2)]))
                    stages[3].append(lambda s2=s2, rA=rA: S.op("act", lambda o, rA=rA: o.activation(out=rA[:, 0:2], in_=rA[:, 0:2], func=AF.Exp, scale=-0.5),
                                                               reads=[("rl0", s2)], writes=[("rl0", s2)]))
                    stages[4].append(lambda s2=s2, rA=rA: S.op("dve", lambda o, s2=s2, rA=rA: o.tensor_tensor(
                        out=t0f[s2], in0=t0f[s2], in1=rA[:, 0:2].unsqueeze(2).broadcast_to([128, 2, 128]), op=ALU.mult),
                        reads=[("t0f", s2), ("rl0", s2)], writes=[("t0f", s2)]))
                    stages[4].append(lambda s2=s2: S.op("dve", lambda o, s2=s2: o.tensor_tensor(
                        out=ob_[s2], in0=t0f[s2], in1=sgb.unsqueeze(1).broadcast_to([128, 2, 128]), op=ALU.mult),
                        reads=[("t0f", s2), "sgb"], writes=[("ob", s2)]))
                    stages[5].append(lambda s2=s2, tr=tr: S.op("pe", tr, reads=[("ob", s2)], writes=[("ps", 7)]))
                    stages[5].append(lambda pv=pv, tq=tq, h=h: S.op("act", lambda o, pv=pv, tq=tq, h=h: o.copy(
                        out=mixT[:, h, tq:tq + 256], in_=pv.rearrange("p c t -> p (c t)")), reads=[("ps", 7)], writes=[("mixT", h, tq)]))
                pending.extend(stages)
                yield

        pending = []
        for _ in proj_gen(0):
            pass
        for h in range(8):
            pg = None
            if h + 1 < 8:
                loadw(h + 1)
                pg = proj_gen(h + 1)
            for tag in attn_gen(h):
                if tag == "mid" and pg is not None:
                    for _k in range(4):
                        try:
                            next(pg)
                        except StopIteration:
                            pg = None
                            break
            if pg is not None:
                for _ in pg:
                    pass
        for f_ in [f for st in pending for f in st]:
            f_()
        del pending[:]
        S.barrier()
        ar.release(m2)
        wo = ar.alloc([128, KC, D], BF16)
        gate = [ar.alloc([128, D], F32) for _ in range(2)]
        tmp = [ar.alloc([128, D], F32) for _ in range(2)]
        wvo = odwout_d[i].rearrange("(k p) n -> p k n", p=128)
        for hh in range(2):
            S.dma("pool", wo[:, :, hh * 512:(hh + 1) * 512], wvo[:, :, hh * 512:(hh + 1) * 512], writes=[("wo", hh)])
        load_mod(gate[0], l, b, 2)
        if need_ctx:
            load_mod(gate[1], l, 2, 2)
        for j in tiles_for(need_ctx):
            ci = 0 if j < NTL else 1
            s2 = j % 2
            b0 = 2 * s2

            def mm(o, j=j, b0=b0):
                r = None
                for hh in range(2):
                    for c in range(KC):
                        r = o.matmul(ps[:, b0 + hh, :], lhsT=mixT[:, c, j * 128:(j + 1) * 128],
                                     rhs=wo[:, c, hh * 512:(hh + 1) * 512], start=(c == 0), stop=(c == KC - 1))
                return r
            S.op("pe", mm, reads=[("wo", 0), ("wo", 1)], writes=[("ps", b0), ("ps", b0 + 1)])
            psv = ps[:, b0:b0 + 2, :].rearrange("p a n -> p (a n)")
            resid_update(j, 0, D, psv, gate[ci], tmp[s2], ("tmp", s2), extra_reads=[("ps", b0), ("ps", b0 + 1)])
        S.barrier()
        ar.release(m)

    def dump_xl(b):
        for j in range(NTL):
            S.dma("sp", out_d[b, j * 128:(j + 1) * 128, :], xl[:, j, :], reads=[("xl", j)])

    def final_phase(b):
        m = ar.mark()
        g = ar.alloc([128, D], F32)
        junk = ar.alloc([128, D], F32)
        o_t = [ar.alloc([128, D], F32) for _ in range(2)]
        S.dma("sp", g, fng_d.partition_broadcast(128), writes=["fg"])
        S.op("dve", lambda o: o.memset(stat, 0.0), writes=["stat"])
        for j in range(NTL):
            s2 = j % 2
            xj = xl[:, j, :]
            S.op("act", lambda o, xj=xj, j=j: o.activation(out=junk, in_=xj, func=AF.Square, accum_out=stat[:, j:j + 1]),
                 reads=[("xl", j), "stat"], writes=["junk", ("stat", j)])
            S.op("act", lambda o, j=j: o.activation(out=stat[:, 32 + j:33 + j], in_=stat[:, j:j + 1], func=AF.Sqrt, bias=1e-6, scale=1.0 / D),
                 reads=[("stat", j)], writes=[("stat2", j)])
            S.op("dve", lambda o, j=j: o.reciprocal(out=stat[:, 32 + j:33 + j], in_=stat[:, 32 + j:33 + j]), reads=[("stat2", j)], writes=[("stat2", j)])
            S.op("dve", lambda o, xj=xj, j=j, s2=s2: o.scalar_tensor_tensor(out=o_t[s2], in0=xj, scalar=stat[:, 32 + j:33 + j], in1=g,
                                                                           op0=ALU.mult, op1=ALU.mult),
                 reads=[("xl", j), ("stat2", j), "fg"], writes=[("ot", s2)])
            S.dma("sp", out_d[b, j * 128:(j + 1) * 128, :], o_t[s2], reads=[("ot", s2)])
        S.barrier()
        ar.release(m)

    for b in range(nb):
        for j in range(NTL):
            S.dma("sp", xl[:, j, :], x_d[b, j * 128:(j + 1) * 128, :], writes=[("xl", j)])
        for j in range(2):
            S.dma("sp", xl[:, NTL + j, :], ctx_d[b, j * 128:(j + 1) * 128, :], writes=[("xl", NTL + j)])
        S.barrier()
        done = False
        for l in range(n_layers):
            need_ctx = l < DEPTH - 1
            norm_phase(l, b, 1, True)
            if stop == ("norm1", l):
                done = True
                break
            if l % 2 == 0:
                even_mixer(l, b, need_ctx)
            else:
                odd_mixer(l, b, need_ctx)
            if stop == ("mix", l):
                done = True
                break
            if l % 2 == 0:
                norm_phase(l, b, 2, need_ctx)
                ffn_phase(l, b, need_ctx, moe=False)
            else:
                norm_phase(l, b, 2, need_ctx, router=l // 2)
                if stop == ("norm2", l):
                    done = True
                    break
                ffn_phase(l, b, need_ctx, moe=True)
        if stop is not None or n_layers < DEPTH:
            dump_xl(b)
        else:
            final_phase(b)
        S.barrier()
    S.finish("sp")
    S.emit()
    pcm.__exit__(None, None, None)
    ar.close()
    return nc


def _rope_tables():
    rows = TL // 64
    r = np.broadcast_to(np.arange(rows, dtype=np.float32)[:, None], (rows, 64)).reshape(-1)
    col = np.broadcast_to(np.arange(64, dtype=np.float32)[None, :], (rows, 64)).reshape(-1)
    inv = (np.float32(10000.0) ** (-np.arange(16, dtype=np.float32) / np.float32(16))).astype(np.float32)
    ang = np.stack([r[:, None] * inv, col[:, None] * inv], axis=1).astype(np.float32)
    return (np.cos(ang).reshape(TL, 32).astype(np.float32), np.sin(ang).reshape(TL, 32).astype(np.float32))


_WEIGHT_KEYS = ["c_ctx", "ada_w", "ada_b", "norm1_g", "norm2_g", "ev_w_in", "ev_conv_w", "ev_ln_g", "ev_ln_b",
                "ev_q_norm_g", "ev_k_norm_g", "ev_w_out", "ev_ffn_wg", "ev_ffn_wu", "ev_ffn_wd", "od_w_in", "od_lam",
                "od_subln_g", "od_w_out", "od_router_w", "od_moe_wg", "od_moe_wu", "od_moe_wd", "final_norm_g"]


def make_in_maps(inputs, cores):
    cos, sin = _rope_tables()
    maps = []
    shared = {k: np.ascontiguousarray(np.asarray(inputs[k], dtype=np.float32)) for k in _WEIGHT_KEYS}
    for c in cores:
        mp = dict(shared)
        mp["x"] = np.ascontiguousarray(np.asarray(inputs["x"][NB * c:NB * (c + 1)], dtype=np.float32))
        mp["c"] = np.ascontiguousarray(np.asarray(inputs["c"][NB * c:NB * (c + 1)], dtype=np.float32))
        mp["ctx"] = np.ascontiguousarray(np.asarray(inputs["ctx"][NB * c:NB * (c + 1)], dtype=np.float32))
        mp["rope_cos"] = cos
        mp["rope_sin"] = sin
        maps.append(mp)
    return maps


def kernel(**inputs):
    nc = build_program()
    cores = list(range(8))
    in_maps = make_in_maps(inputs, cores)
    res = run_bass_kernel_spmd(nc, in_maps, core_ids=cores)
    out = np.concatenate([np.asarray(r["out"], dtype=np.float32) for r in res.results], axis=0)
    return out
```
